# Optimizing a Trainium2 kernel written in Bass

```python
import jax, jax.numpy as jnp
from jax import lax
import numpy as np

D_MODEL = 1024
BATCH = 8
SEQ = 2048
DEPTH = 2

N_HEADS = 16
HEAD_DIM = D_MODEL // N_HEADS
Q_BLOCK = 128
N_A_LAYERS = DEPTH // 2
N_B_LAYERS = DEPTH - N_A_LAYERS
N_GROUPS = 4
EXPERTS_PER_GROUP = 4
N_EXPERTS = N_GROUPS * EXPERTS_PER_GROUP
TOP_K_IN_GROUP = 2
D_EXPERT = D_MODEL // 2
RMS_EPS = 1e-6

kernel_name = "yoco_stickbreak_fox_hmoe"


def rms_norm(x, g):
    x32 = x.astype(jnp.float32)
    y = x32 * lax.rsqrt(jnp.mean(x32 * x32, axis=-1, keepdims=True) + RMS_EPS)
    return (y * g.astype(jnp.float32)).astype(x.dtype)


def split_heads(t):
    b, s, _ = t.shape
    return t.reshape(b, s, N_HEADS, HEAD_DIM).transpose(0, 2, 1, 3)


def merge_heads(t):
    b, h, s, dh = t.shape
    return t.transpose(0, 2, 1, 3).reshape(b, s, h * dh)


def stick_breaking_attention(q, k, v):
    seq = q.shape[2]
    scale = HEAD_DIM ** -0.5
    outs = []
    for i in range(seq // Q_BLOCK):
        q0 = i * Q_BLOCK
        kv_len = q0 + Q_BLOCK
        qb = q[:, :, q0:kv_len].astype(jnp.float32)
        kb = k[:, :, :kv_len].astype(jnp.float32)
        vb = v[:, :, :kv_len].astype(jnp.float32)
        z = jnp.einsum('bhqd,bhkd->bhqk', qb, kb) * scale
        t_pos = q0 + jnp.arange(Q_BLOCK)[:, None]
        s_pos = jnp.arange(kv_len)[None, :]
        strict = s_pos < t_pos
        log_keep = jnp.where(strict, jax.nn.log_sigmoid(-z), 0.0)
        log_after = lax.cumsum(log_keep, axis=3, reverse=True) - log_keep
        w = jnp.where(strict, jnp.exp(jax.nn.log_sigmoid(z) + log_after), 0.0)
        outs.append(jnp.einsum('bhqk,bhkd->bhqd', w, vb))
    return jnp.concatenate(outs, axis=2).astype(q.dtype)


def forgetting_attention(q, k, v, cum_logf):
    seq = q.shape[2]
    scale = HEAD_DIM ** -0.5
    outs = []
    for i in range(seq // Q_BLOCK):
        q0 = i * Q_BLOCK
        kv_len = q0 + Q_BLOCK
        qb = q[:, :, q0:kv_len].astype(jnp.float32)
        kb = k[:, :, :kv_len].astype(jnp.float32)
        vb = v[:, :, :kv_len].astype(jnp.float32)
        logits = (jnp.einsum('bhqd,bhkd->bhqk', qb, kb) * scale
                  + cum_logf[:, :, q0:kv_len, None] - cum_logf[:, :, None, :kv_len])
        t_pos = q0 + jnp.arange(Q_BLOCK)[:, None]
        s_pos = jnp.arange(kv_len)[None, :]
        probs = jax.nn.softmax(jnp.where(s_pos <= t_pos, logits, -jnp.inf), axis=-1)
        outs.append(jnp.einsum('bhqk,bhkd->bhqd', probs, vb))
    return jnp.concatenate(outs, axis=2).astype(q.dtype)


def hierarchical_moe(x, w_group, w_router, w_gate, w_up, w_down):
    b, s, d = x.shape
    xt = x.reshape(b * s, d)
    group_probs = jax.nn.softmax((xt @ w_group).astype(jnp.float32), axis=-1)
    g_prob, g_idx = lax.top_k(group_probs, 1)
    expert_logits = (xt @ w_router).astype(jnp.float32).reshape(-1, N_GROUPS, EXPERTS_PER_GROUP)
    in_group = jnp.take_along_axis(expert_logits, g_idx[:, :, None], axis=1)[:, 0]
    top_logits, e_idx = lax.top_k(in_group, TOP_K_IN_GROUP)
    e_w = jax.nn.softmax(top_logits, axis=-1) * g_prob
    expert_id = g_idx * EXPERTS_PER_GROUP + e_idx
    combine = jnp.sum(jax.nn.one_hot(expert_id, N_EXPERTS, dtype=jnp.float32) * e_w[..., None], axis=1)
    h = jax.nn.silu(jnp.einsum('nd,edf->nef', xt, w_gate)) * jnp.einsum('nd,edf->nef', xt, w_up)
    h = h * combine[:, :, None].astype(h.dtype)
    y = jnp.einsum('nef,efd->nd', h, w_down)
    return y.reshape(b, s, d).astype(x.dtype)


def setup_inputs(seed: int = 0) -> dict:
    key = jax.random.key(seed)
    ks = jax.random.split(key, 20)
    d, h, f = D_MODEL, N_HEADS, D_EXPERT
    sd = d ** -0.5
    nrm = jax.random.normal
    return {
        "x": nrm(ks[0], (BATCH, SEQ, d), jnp.float32),
        "attn_norm": 1.0 + 0.02 * nrm(ks[1], (DEPTH, d), jnp.float32),
        "w_qkv_a": nrm(ks[2], (N_A_LAYERS, d, 3 * d), jnp.float32) * sd,
        "w_o_a": nrm(ks[3], (N_A_LAYERS, d, d), jnp.float32) * sd,
        "kv_norm": 1.0 + 0.02 * nrm(ks[4], (d,), jnp.float32),
        "w_kvf": nrm(ks[5], (d, 2 * d + h), jnp.float32) * sd,
        "b_f": jax.random.uniform(ks[6], (h,), jnp.float32, 1.0, 4.0),
        "w_q_b": nrm(ks[7], (N_B_LAYERS, d, d), jnp.float32) * sd,
        "w_o_b": nrm(ks[8], (N_B_LAYERS, d, d), jnp.float32) * sd,
        "moe_norm": 1.0 + 0.02 * nrm(ks[9], (DEPTH, d), jnp.float32),
        "w_group": nrm(ks[10], (DEPTH, d, N_GROUPS), jnp.float32) * sd,
        "w_router": nrm(ks[11], (DEPTH, d, N_EXPERTS), jnp.float32) * sd,
        "w_gate": nrm(ks[12], (DEPTH, N_EXPERTS, d, f), jnp.float32) * sd,
        "w_up": nrm(ks[13], (DEPTH, N_EXPERTS, d, f), jnp.float32) * sd,
        "w_down": nrm(ks[14], (DEPTH, N_EXPERTS, f, d), jnp.float32) * (f ** -0.5),
        "final_norm": 1.0 + 0.02 * nrm(ks[15], (d,), jnp.float32),
    }


def reference(x, attn_norm, w_qkv_a, w_o_a, kv_norm, w_kvf, b_f, w_q_b, w_o_b,
              moe_norm, w_group, w_router, w_gate, w_up, w_down, final_norm):
    d = D_MODEL
    h = x
    k_shared = v_shared = cum_logf = None
    for layer in range(DEPTH):
        if layer < N_A_LAYERS:
            hn = rms_norm(h, attn_norm[layer])
            q, k, v = jnp.split(hn @ w_qkv_a[layer], 3, axis=-1)
            mix = stick_breaking_attention(split_heads(q), split_heads(k), split_heads(v))
            h = h + merge_heads(mix) @ w_o_a[layer]
        else:
            j = layer - N_A_LAYERS
            if j == 0:
                u = rms_norm(h, kv_norm)
                kvf = u @ w_kvf
                k_shared = split_heads(kvf[..., :d])
                v_shared = split_heads(kvf[..., d:2 * d])
                log_f = jax.nn.log_sigmoid((kvf[..., 2 * d:] + b_f).astype(jnp.float32))
                cum_logf = lax.cumsum(log_f, axis=1).transpose(0, 2, 1)
            hn = rms_norm(h, attn_norm[layer])
            q = split_heads(hn @ w_q_b[j])
            mix = forgetting_attention(q, k_shared, v_shared, cum_logf)
            h = h + merge_heads(mix) @ w_o_b[j]
        h = h + hierarchical_moe(rms_norm(h, moe_norm[layer]), w_group[layer], w_router[layer],
                                 w_gate[layer], w_up[layer], w_down[layer])
    return rms_norm(h, final_norm)
```

```python
from contextlib import ExitStack

import numpy as np
import concourse.bass as bass
import concourse.mybir as mybir
from concourse.bass_utils import run_bass_kernel_spmd

F32 = mybir.dt.float32
BF16 = mybir.dt.bfloat16
AF = mybir.ActivationFunctionType
ALU = mybir.AluOpType

D = 1024
S = 2048
NT = S // 128
DC = D // 128
H = 16
DH = 64
NE = 16
FE = 512
EPS = 1e-6
NEG = -30000.0
ARENA = 34688
FARENA = 5760
ENGS = ("pe", "act", "dve", "pool", "sp")


class Prog:
    def __init__(self):
        self.ops = []

    def op(self, eng, fn, reads=(), writes=()):
        self.ops.append(dict(eng=eng, fn=fn, r=tuple(reads), w=tuple(writes), kind="op", prod=eng))

    def dma(self, q, fn, reads=(), writes=(), lane=None):
        assert lane is not None
        self.ops.append(dict(eng=q, fn=fn, r=tuple(reads), w=tuple(writes), kind="dma", prod="L:" + lane))

    def barrier(self):
        for e in ENGS:
            self.ops.append(dict(eng=e, fn=None, r=(), w=(), kind="bar", prod=None))

    def wait_all(self, eng, reads):
        self.ops.append(dict(eng=eng, fn=None, r=tuple(reads), w=(), kind="wait", prod=None))

    def schedule(self):
        ops = self.ops
        last_w, readers = {}, {}
        seq, cnt = [], {}
        waited = {e: {} for e in ENGS}
        last_in = {}
        signal = set()
        for i, o in enumerate(ops):
            p = o["prod"]
            if p is not None:
                cnt[p] = cnt.get(p, 0) + 1
                seq.append(cnt[p])
            else:
                seq.append(None)
            ce = o["eng"]
            raw = set()
            if o["kind"] == "bar":
                raw = set(last_in.values())
            else:
                for k in o["r"]:
                    if k in last_w:
                        raw.add(last_w[k])
                for k in o["w"]:
                    if k in last_w:
                        raw.add(last_w[k])
                    raw.update(readers.get(k, ()))
            need = {}
            for j in raw:
                pj = ops[j]["prod"]
                if pj == "pe" and ce == "pe" and o["kind"] == "op":
                    continue
                if waited[ce].get(pj, 0) >= seq[j]:
                    continue
                if pj not in need or seq[j] > seq[need[pj]]:
                    need[pj] = j
            o["waits"] = []
            for pj, j in need.items():
                o["waits"].append(j)
                waited[ce][pj] = seq[j]
                signal.add(j)
            for k in o["w"]:
                last_w[k] = i
                readers[k] = []
            for k in o["r"]:
                readers.setdefault(k, []).append(i)
            if p is not None:
                last_in[p] = i
        val, run = {}, {}
        for i, o in enumerate(ops):
            p = o["prod"]
            if p is None:
                continue
            if i in signal:
                run[p] = run.get(p, 0) + (16 if o["kind"] == "dma" else 1)
            val[i] = run.get(p, 0)
        self.signal, self.val = signal, val
        self.prods = sorted(cnt.keys())

    def emit(self, nc):
        self.schedule()
        ops = self.ops
        with ExitStack() as es:
            sems = {}
            for p in self.prods:
                sems[p] = es.enter_context(nc.semaphore("s_" + p.replace(":", "_")))
            block = es.enter_context(nc.Block())

            def run(engname, e):
                for i, o in enumerate(ops):
                    if o["eng"] != engname:
                        continue
                    for j in o["waits"]:
                        e.wait_ge(sems[ops[j]["prod"]], self.val[j])
                    if o["fn"] is not None:
                        ins = o["fn"](e)
                        if i in self.signal:
                            ins.then_inc(sems[o["prod"]], 16 if o["kind"] == "dma" else 1)

            @block.tensor
            def _(e):
                run("pe", e)

            @block.scalar
            def _(e):
                run("act", e)

            @block.vector
            def _(e):
                run("dve", e)

            @block.gpsimd
            def _(e):
                run("pool", e)

            @block.sync
            def _(e):
                run("sp", e)


def build(nc, cfg):
    P = Prog()
    es = ExitStack()
    dr = {}

    def din(name, shape):
        dr[name] = nc.dram_tensor(name, list(shape), F32, kind="ExternalInput").ap()
        return dr[name]

    x = din("x", (S, D))
    attn_norm = din("attn_norm", (2, D))
    w_qkv_a = din("w_qkv_a", (D, 3 * D))
    w_o_a = din("w_o_a", (D, D))
    kv_norm = din("kv_norm", (1, D))
    w_kvf = din("w_kvf", (D, 2 * D + H))
    b_f = din("b_f", (1, H))
    w_q_b = din("w_q_b", (D, D))
    w_o_b = din("w_o_b", (D, D))
    moe_norm = din("moe_norm", (2, D))
    w_group = din("w_group", (2, D, 4))
    w_router = din("w_router", (2, D, NE))
    w_gate = din("w_gate", (2, NE, D, FE))
    w_up = din("w_up", (2, NE, D, FE))
    w_down = din("w_down", (2, NE, FE, D))
    final_norm = din("final_norm", (1, D))
    out = nc.dram_tensor("out", [S, D], F32, kind="ExternalOutput").ap()

    def sb(name, shape, dt):
        return es.enter_context(nc.sbuf_tensor(name, list(shape), dt))

    h = sb("h", (128, NT, D), F32)
    bufA = sb("bufA", (128, DC, S), BF16)
    arena = sb("arena", (128, ARENA), BF16)
    farena = sb("farena", (128, FARENA), F32)
    xs = sb("xs", (128, 2, 4, D), BF16)
    sqj = sb("sqj", (128, D), BF16)
    ss = sb("ss", (128, NT), F32)
    lnv = sb("lnv", (128, NT), F32)
    rstd = sb("rstd", (128, NT), F32)
    gcols = sb("gcols", (128, 5, DC), F32)
    ident = sb("ident", (128, 128), BF16)
    onesb = sb("onesb", (128, 128), BF16)
    onesf = sb("onesf", (128, 128), F32)
    ps = [es.enter_context(nc.psum_tensor("ps%d" % k, [128, 512], F32)) for k in range(8)]

    def PS(k):
        return ("ps", k)

    P.op("pool", lambda e: e.memset(onesb[:], 1.0), writes=["onesb"])
    P.op("pool", lambda e: e.memset(onesf[:], 1.0), writes=["onesf"])
    P.op("pool", lambda e: e.affine_select(out=ident[:], in_=onesb[:], pattern=[[-1, 128]],
                                            compare_op=ALU.is_equal, fill=0.0, base=0, channel_multiplier=1),
         reads=["onesb"], writes=["ident"])
    gsrc = [attn_norm[0:1, :], attn_norm[1:2, :], kv_norm[0:1, :], moe_norm[0:1, :], moe_norm[1:2, :]]
    for n, g in enumerate(gsrc):
        P.dma("act", (lambda e, n=n, g=g: e.dma_start(
            out=gcols[:, n, :], in_=g.rearrange("o (c p) -> p (o c)", p=128), allow_slow_non_contiguous=True)),
            writes=[("gcols", n)], lane="g%d" % n)
    xv = x.rearrange("(t p) d -> p t d", p=128)
    for q in range(4):
        P.dma("sp", (lambda e, q=q: e.dma_start(out=h[:, 4 * q:4 * q + 4, :], in_=xv[:, 4 * q:4 * q + 4, :])),
              writes=[("h", t) for t in range(4 * q, 4 * q + 4)], lane="x%d" % q)

    def norm_stats(t0, t1):
        for t in range(t0, t1):
            P.op("act", (lambda e, t=t: e.activation(out=sqj[:], in_=h[:, t, :], func=AF.Square,
                                                      accum_out=ss[:, t:t + 1])),
                 reads=[("h", t)], writes=["sqj", ("ss", t)])
        P.op("act", (lambda e: e.activation(out=lnv[:, t0:t1], in_=ss[:, t0:t1], func=AF.Ln,
                                            bias=EPS, scale=1.0 / D)),
             reads=[("ss", t) for t in range(t0, t1)], writes=[("lnv", t) for t in range(t0, t1)])
        P.op("act", (lambda e: e.activation(out=rstd[:, t0:t1], in_=lnv[:, t0:t1], func=AF.Exp, scale=-0.5)),
             reads=[("lnv", t) for t in range(t0, t1)], writes=[("rstd", t) for t in range(t0, t1)])

    def norm_group_a(tg, extra_w=()):
        sl = tg % 2
        norm_stats(4 * tg, 4 * tg + 4)
        for i in range(4):
            t = 4 * tg + i
            P.op("dve", (lambda e, t=t, i=i: e.tensor_scalar(
                out=xs[:, sl, i, :], in0=h[:, t, :], scalar1=rstd[:, t:t + 1], scalar2=None, op0=ALU.mult)),
                reads=[("h", t), ("rstd", t)], writes=[("xs", sl, i)] + list(extra_w))

    def norm_group_b(tg, gain_idx, banks=(0, 1, 2, 3), chunks=range(DC)):
        sl = tg % 2
        for c in chunks:
            k = banks[c % len(banks)]
            for i in range(4):
                P.op("pe", (lambda e, k=k, i=i, c=c: e.matmul(
                    ps[k][:, i * 128:(i + 1) * 128], lhsT=xs[:, sl, i, c * 128:(c + 1) * 128],
                    rhs=ident[:], start=True, stop=True)),
                    reads=[("xs", sl, i), "ident"], writes=[PS(k)])
            dst = bufA[:, c, tg * 512:(tg + 1) * 512]
            if gain_idx is None:
                if c % 2 == 0:
                    P.op("act", (lambda e, k=k, dst=dst: e.copy(out=dst, in_=ps[k][:])),
                         reads=[PS(k)], writes=[("A", c, tg)])
                else:
                    P.op("dve", (lambda e, k=k, dst=dst: e.tensor_copy(out=dst, in_=ps[k][:])),
                         reads=[PS(k)], writes=[("A", c, tg)])
            else:
                gc = gcols[:, gain_idx, c:c + 1]
                if c % 2 == 0:
                    P.op("act", (lambda e, k=k, dst=dst, gc=gc: e.activation(
                        out=dst, in_=ps[k][:], func=AF.Copy, scale=gc)),
                        reads=[PS(k), ("gcols", gain_idx)], writes=[("A", c, tg)])
                else:
                    P.op("dve", (lambda e, k=k, dst=dst, gc=gc: e.tensor_scalar(
                        out=dst, in0=ps[k][:], scalar1=gc, scalar2=None, op0=ALU.mult)),
                        reads=[PS(k), ("gcols", gain_idx)], writes=[("A", c, tg)])

    def norm_group(tg, gain_idx, banks=(0, 1, 2, 3), extra_w=()):
        norm_group_a(tg, extra_w)
        norm_group_b(tg, gain_idx, banks)

    def norm_transpose(gain_idx):
        for tg in range(4):
            norm_group(tg, gain_idx)

    A_ALL = [("A", c, tg) for c in range(DC) for tg in range(4)]


    def attention(layer, prenormed=False):
        fox = (layer == 1)
        P.barrier()
        mixT = arena[:, 0:16384].rearrange("p (c t) -> p c t", c=DC)
        o = 16384
        QTz = [arena[:, o + i * 2048:o + (i + 1) * 2048] for i in range(2)]
        o += 4096
        if not fox:
            KT = arena[:, o:o + 2048]
            KTz = [KT, KT]
            o += 2048
        else:
            KTz = [arena[:, o + i * 2048:o + (i + 1) * 2048] for i in range(2)]
            o += 4096
        Vz = [arena[:, o + i * 2048:o + (i + 1) * 2048].rearrange("p (t d) -> p t d", t=NT) for i in range(2)]
        o += 4096
        wq = arena[:, o:o + 1024].rearrange("p (c n) -> p c n", c=DC)
        wk = arena[:, o + 1024:o + 2048].rearrange("p (c n) -> p c n", c=DC)
        wv = arena[:, o + 2048:o + 3072].rearrange("p (c n) -> p c n", c=DC)
        o += 3072
        wb = [arena[:, o + i * 512:o + (i + 1) * 512] for i in range(3)]
        o += 1536
        tri = arena[:, o:o + 128]
        o += 128
        xsf = xs[:].rearrange("p s a d -> p (s a d)")
        if not fox:
            triu = arena[:, o:o + 128]
            mstrb = arena[:, o + 128:o + 256]
            o += 256
            spb = [arena[:, o + i * 512:o + (i + 1) * 512] for i in range(3)]
            o += 1536
        else:
            nhi = xsf[0:16, 0:2048]
            nlo = xsf[0:16, 2048:4096]
            wfb = arena[:, o:o + 128].rearrange("p (c n) -> p c n", c=DC)
            negb = arena[:, o + 128:o + 256]
            o += 256
            r_hi = [arena[64:65, o:o + 512], arena[0:1, o:o + 512]]
            r_lo = [arena[64:65, o + 512:o + 1024], arena[0:1, o + 512:o + 1024]]
            o += 1024
        assert o <= ARENA
        Q0 = [[QTz[hf][:, 0:512] for hf in range(2)], [xsf[:, 4096 + hf * 512:4096 + (hf + 1) * 512] for hf in range(2)]]
        if not fox:
            K0 = [[KT[:, 0:512]] * 2, [xsf[:, 5120:5632]] * 2]
        else:
            K0 = [[KTz[hf][:, 0:512] for hf in range(2)], [xsf[:, 5120 + hf * 512:5120 + (hf + 1) * 512] for hf in range(2)]]
        V0 = [[Vz[hf][:, 0:4, :] for hf in range(2)],
              [xsf[:, 6144 + hf * 512:6144 + (hf + 1) * 512].rearrange("p (t d) -> p t d", t=4) for hf in range(2)]]

        def Qap(par, hf, qg, lo):
            return Q0[par][hf][:, lo:512] if qg == 0 else QTz[hf][:, qg * 512 + lo:(qg + 1) * 512]

        def Kap(par, hf, a):
            return K0[par][hf][:, a * 128:(a + 1) * 128] if a < 4 else KTz[hf][:, a * 128:(a + 1) * 128]

        def Vap(par, hf, a):
            return V0[par][hf][:, a, :] if a < 4 else Vz[hf][:, a, :]

        def Qk(par, qg):
            return ("QT0", par) if qg == 0 else ("QT", qg)

        def Kk(par, a):
            return ("KT0", par) if a < 4 else ("KT", a // 4)

        def Vk(par, a):
            return ("V0", par) if a < 4 else ("V", a // 4)
        stg = [farena[:, i * 1024:(i + 1) * 1024].rearrange("p (c n) -> p c n", c=DC) for i in range(2)]
        fo = 2048
        if not fox:
            ef = [farena[:, fo + i * 512:fo + (i + 1) * 512] for i in range(5)]
            spf = [farena[:, fo + 2560 + i * 512:fo + 2560 + (i + 1) * 512] for i in range(2)]
            fo += 3584
            mstrict = farena[:, fo:fo + 128]
            fo += 128
        else:
            uc = farena[:, fo:fo + 1408]
            fo += 1408
            nlf = farena[:, fo:fo + 256].rearrange("p (t h) -> p t h", t=NT)
            ncum = farena[:, fo + 256:fo + 512].rearrange("p (t h) -> p t h", t=NT)
            fo += 512
            bfb = farena[:, fo:fo + 16]
            fo += 16
            spre = farena[:, fo:fo + 256].rearrange("p (t h) -> p t h", t=NT)
            fo += 256
            r_sb = [farena[64:65, fo:fo + 512], farena[0:1, fo:fo + 512]]
            bc_sb = [farena[0:64, fo + 512:fo + 1024], farena[64:128, fo + 512:fo + 1024]]
            fo += 1024
            negm2 = farena[:, fo:fo + 128]
            fo += 128
            stf = farena[:, fo:fo + 128].rearrange("p (c n) -> p c n", c=DC)
            fo += 128
        assert fo <= FARENA

        if not prenormed:
            norm_transpose(None)

        P.op("pool", lambda e: e.affine_select(out=tri, in_=onesb[:], pattern=[[-1, 128]], compare_op=ALU.is_gt,
                                                fill=0.0, base=0, channel_multiplier=1),
             reads=["onesb"], writes=["tri"])
        if not fox:
            P.op("pool", lambda e: e.affine_select(out=mstrict, in_=onesf[:], pattern=[[1, 128]],
                                                    compare_op=ALU.is_gt, fill=0.0, base=0, channel_multiplier=-1),
                 reads=["onesf"], writes=["mstrict"])
            P.op("pool", lambda e: e.tensor_copy(out=mstrb, in_=mstrict), reads=["mstrict"], writes=["mstrb"])
            P.op("pool", lambda e: e.affine_select(out=triu, in_=onesb[:], pattern=[[1, 128]], compare_op=ALU.is_ge,
                                                    fill=0.0, base=0, channel_multiplier=-1),
                 reads=["onesb"], writes=["triu"])
        else:
            P.op("pool", lambda e: e.affine_select(out=negm2, in_=onesf[:], pattern=[[1, 128]],
                                                    compare_op=ALU.is_ge, fill=0.0, base=0, channel_multiplier=-1),
                 reads=["onesf"], writes=["negm2"])
            P.op("pool", lambda e: e.tensor_scalar(out=negm2, in0=negm2, scalar1=-1.0, scalar2=-NEG,
                                                   op0=ALU.add, op1=ALU.mult),
                 reads=["negm2"], writes=["negm2"])
            P.op("pool", lambda e: e.tensor_copy(out=negb, in_=negm2), reads=["negm2"], writes=["negb"])
            P.op("pool", lambda e: e.memset(uc, 1.0), writes=["uc"])
            P.op("pool", lambda e: e.affine_select(out=uc, in_=uc, pattern=[[1, 1408]], compare_op=ALU.is_ge,
                                                    fill=0.0, base=-384, channel_multiplier=-1),
                 reads=["uc"], writes=["uc"])
            P.dma("sp", lambda e: e.dma_start(out=bfb, in_=b_f.partition_broadcast(128)), writes=["bfb"], lane="bfb")

        P.op("dve", lambda e: e.memset(QTz[0][64:128, :], 0.0), writes=["pad"])
        P.op("dve", lambda e: e.memset(QTz[1][0:64, :], 0.0), writes=["pad"])
        P.op("dve", lambda e: e.memset(Vz[0][:, :, 64:128], 0.0), writes=["pad"])
        P.op("dve", lambda e: e.memset(Vz[1][:, :, 0:64], 0.0), writes=["pad"])
        if fox:
            P.op("dve", lambda e: e.memset(KTz[0][64:128, :], 0.0), writes=["pad"])
            P.op("dve", lambda e: e.memset(KTz[1][0:64, :], 0.0), writes=["pad"])
            P.op("dve", lambda e: e.memset(KTz[0][64:66, :], 1.0), writes=["pad"])
            P.op("dve", lambda e: e.memset(KTz[1][0:2, :], 1.0), writes=["pad"])
            P.op("dve", lambda e: e.memset(Vz[0][:, :, 64:65], 1.0), writes=["pad"])
            P.op("dve", lambda e: e.memset(Vz[1][:, :, 0:1], 1.0), writes=["pad"])

        XS1 = [("xs", 1, i) for i in range(4)]
        P.op("dve", lambda e: e.memset(Q0[1][0][64:128, :], 0.0), writes=["pad"] + XS1)
        P.op("dve", lambda e: e.memset(Q0[1][1][0:64, :], 0.0), writes=["pad"] + XS1)
        P.op("dve", lambda e: e.memset(V0[1][0][:, :, 64:128], 0.0), writes=["pad"] + XS1)
        P.op("dve", lambda e: e.memset(V0[1][1][:, :, 0:64], 0.0), writes=["pad"] + XS1)
        if fox:
            P.op("dve", lambda e: e.memset(K0[1][0][64:128, :], 0.0), writes=["pad"] + XS1)
            P.op("dve", lambda e: e.memset(K0[1][1][0:64, :], 0.0), writes=["pad"] + XS1)
            P.op("dve", lambda e: e.memset(K0[1][0][64:66, :], 1.0), writes=["pad"] + XS1)
            P.op("dve", lambda e: e.memset(K0[1][1][0:2, :], 1.0), writes=["pad"] + XS1)
            P.op("dve", lambda e: e.memset(V0[1][0][:, :, 64:65], 1.0), writes=["pad"] + XS1)
            P.op("dve", lambda e: e.memset(V0[1][1][:, :, 0:1], 1.0), writes=["pad"] + XS1)

        def load_fold(src_ap, gi, dst, sl, width, key):
            sv = src_ap.rearrange("(c p) n -> p c n", p=128)
            st = stg[sl][:, :, 0:width] if width == 128 else stf
            skey = ("stg", sl) if width == 128 else "stf"
            P.dma("sp", (lambda e: e.dma_start(out=st, in_=sv)), writes=[skey], lane="stg%d" % sl if width == 128 else "stf")
            for c in range(DC):
                P.op("pool", (lambda e, c=c: e.tensor_scalar(out=dst[:, c, :], in0=st[:, c, :],
                                                             scalar1=gcols[:, gi, c:c + 1], scalar2=1.0,
                                                             op0=ALU.mult, op1=ALU.mult)),
                     reads=[skey, ("gcols", gi)], writes=[key])

        if fox:
            load_fold(w_kvf[:, 2 * D:2 * D + H], 2, wfb, 0, 16, "wfb")
            for q in range(4):
                k = q % 2
                for i in range(4):
                    t = 4 * q + i
                    for c in range(DC):
                        P.op("pe", (lambda e, k=k, i=i, t=t, c=c: e.matmul(
                            ps[k][:, i * 16:(i + 1) * 16], lhsT=bufA[:, c, t * 128:(t + 1) * 128], rhs=wfb[:, c, :],
                            start=(c == 0), stop=(c == DC - 1))),
                            reads=[("A", c, q), "wfb"], writes=[PS(k)])
                P.op("dve", (lambda e, k=k, q=q: e.tensor_tensor(
                    out=nlf[:, 4 * q:4 * q + 4, :], in0=ps[k][:, 0:64].rearrange("p (t h) -> p t h", t=4),
                    in1=bfb.unsqueeze(1).to_broadcast([128, 4, 16]), op=ALU.add)),
                    reads=[PS(k), "bfb"], writes=[("nlf", q)])
            NLF = [("nlf", q) for q in range(4)]
            nlf2 = farena[:, 2048 + 1408:2048 + 1408 + 256]
            P.op("act", lambda e: e.activation(out=nlf2, in_=nlf2, func=AF.Exp, scale=-1.0), reads=NLF, writes=NLF)
            P.op("act", lambda e: e.activation(out=nlf2, in_=nlf2, func=AF.Ln, bias=1.0), reads=NLF, writes=NLF)
            P.op("dve", lambda e: e.memset(spre[:, 0, :], 0.0), writes=[("spre", 0)])
            for t in range(1, NT):
                P.op("dve", (lambda e, t=t: e.tensor_tensor(out=spre[:, t, :], in0=spre[:, t - 1, :],
                                                            in1=nlf[:, t - 1, :], op=ALU.add)),
                     reads=NLF + [("spre", t - 1)], writes=[("spre", t)])
            SPRE = [("spre", t) for t in range(NT)]
            for tg in range(4):
                P.op("pe", (lambda e, tg=tg: e.matmul(ps[7][0:16, :], lhsT=spre[:, 4 * tg, :], rhs=uc[:, 896:1408],
                                                      start=True, stop=False)),
                     reads=SPRE + ["uc"], writes=[PS(7)])
                for dl in range(4):
                    off = 384 - 128 * dl
                    P.op("pe", (lambda e, tg=tg, dl=dl, off=off: e.matmul(
                        ps[7][0:16, :], lhsT=nlf[:, 4 * tg + dl, :], rhs=uc[:, off:off + 512],
                        start=False, stop=(dl == 3))),
                        reads=NLF + ["uc"], writes=[PS(7)])
                cs = slice(tg * 512, (tg + 1) * 512)
                P.op("dve", (lambda e, cs=cs: e.tensor_scalar(out=nhi[:, cs], in0=ps[7][0:16, :], scalar1=-1.0,
                                                              scalar2=None, op0=ALU.mult)),
                     reads=[PS(7)], writes=[("nhi", tg)])
                P.op("dve", (lambda e, cs=cs: e.scalar_tensor_tensor(out=nlo[:, cs], in0=ps[7][0:16, :], scalar=-1.0,
                                                                     in1=nhi[:, cs], op0=ALU.mult, op1=ALU.subtract)),
                     reads=[PS(7), ("nhi", tg)], writes=[("nlo", tg)])
            for q in range(4):
                k = 4 + q % 2
                for i in range(4):
                    t = 4 * q + i
                    P.op("pe", (lambda e, k=k, i=i, t=t: e.matmul(
                        ps[k][:, i * 16:(i + 1) * 16], lhsT=onesf[:], rhs=spre[:, t, :], start=True, stop=False)),
                        reads=SPRE + ["onesf"], writes=[PS(k)])
                    P.op("pe", (lambda e, k=k, i=i, t=t: e.matmul(
                        ps[k][:, i * 16:(i + 1) * 16], lhsT=uc[:, 384:512], rhs=nlf[:, t, :], start=False, stop=True)),
                        reads=NLF + ["uc"], writes=[PS(k)])
                P.op("act", (lambda e, k=k, q=q: e.copy(out=ncum[:, 4 * q:4 * q + 4, :],
                                                        in_=ps[k][:, 0:64].rearrange("p (t h) -> p t h", t=4))),
                     reads=[PS(k)], writes=[("ncum", q)])

        def proj_groups(j):
            par = j % 2
            cs = slice(128 * j, 128 * j + 128)
            if not fox:
                srcs = [(w_qkv_a[:, cs], 0), (w_qkv_a[:, D + 128 * j:D + 128 * j + 128], 0),
                        (w_qkv_a[:, 2 * D + 128 * j:2 * D + 128 * j + 128], 0)]
            else:
                srcs = [(w_q_b[:, cs], 1), (w_kvf[:, cs], 2), (w_kvf[:, D + 128 * j:D + 128 * j + 128], 2)]

            def aug(part):
                for hf in range(2):
                    hg = 2 * j + hf
                    r0 = 64 if hf == 0 else 0
                    for r, srcrow, ln in ((r0, nhi, "a"), (r0 + 1, nlo, "b")):
                        if part == 0:
                            dst, sc, key = Q0[par][hf][r:r + 1, :], slice(0, 512), ("QTaug0", par, hf)
                        else:
                            dst, sc, key = QTz[hf][r:r + 1, 512:2048], slice(512, 2048), ("QTaug", hf)
                        P.dma("sp", (lambda e, dst=dst, sc=sc, hg=hg, srcrow=srcrow: e.dma_start(
                            out=dst, in_=srcrow[hg:hg + 1, sc])),
                            reads=[("nhi", q) for q in range(4)] + [("nlo", q) for q in range(4)], writes=[key],
                            lane="aug%d%s%d" % (hf, ln, part))

            def prologue():
                load_fold(srcs[0][0], srcs[0][1], wq, 0, 128, "wq")
                load_fold(srcs[1][0], srcs[1][1], wk, 1, 128, "wk")
                load_fold(srcs[2][0], srcs[2][1], wv, 0, 128, "wv")
                if fox:
                    aug(0)

            def gq(tg, bq):
                tcs = slice(tg * 512, (tg + 1) * 512)
                for c in range(DC):
                    P.op("pe", (lambda e, c=c: e.matmul(ps[bq][:], lhsT=wq[:, c, :], rhs=bufA[:, c, tcs],
                                                        start=(c == 0), stop=(c == DC - 1))),
                         reads=["wq", ("A", c, tg)], writes=[PS(bq)])
                for hf in range(2):
                    rs = slice(64 * hf, 64 * hf + 64)
                    dst = Q0[par][hf][rs, :] if tg == 0 else QTz[hf][rs, tcs]
                    if fox:
                        P.op("dve", (lambda e, rs=rs, dst=dst: e.tensor_scalar(out=dst, in0=ps[bq][rs, :],
                                                                               scalar1=DH ** -0.5, scalar2=None,
                                                                               op0=ALU.mult)),
                             reads=[PS(bq)], writes=[Qk(par, tg)])
                    else:
                        P.op("act", (lambda e, rs=rs, dst=dst: e.mul(out=dst, in_=ps[bq][rs, :], mul=DH ** -0.5)),
                             reads=[PS(bq)], writes=[Qk(par, tg)])

            def gk(tg, bk):
                tcs = slice(tg * 512, (tg + 1) * 512)
                for c in range(DC):
                    P.op("pe", (lambda e, c=c: e.matmul(ps[bk][:], lhsT=wk[:, c, :], rhs=bufA[:, c, tcs],
                                                        start=(c == 0), stop=(c == DC - 1))),
                         reads=["wk", ("A", c, tg)], writes=[PS(bk)])
                kkey = ("KT0", par) if tg == 0 else ("KT", tg)
                if not fox:
                    dst = K0[par][0] if tg == 0 else KT[:, tcs]
                    P.op("dve", (lambda e: e.tensor_copy(out=dst, in_=ps[bk][:])), reads=[PS(bk)], writes=[kkey])
                else:
                    for hf in range(2):
                        rs = slice(64 * hf, 64 * hf + 64)
                        dst = K0[par][hf][rs, :] if tg == 0 else KTz[hf][rs, tcs]
                        P.op("dve", (lambda e, rs=rs, dst=dst: e.tensor_copy(out=dst, in_=ps[bk][rs, :])),
                             reads=[PS(bk)], writes=[kkey])

            def gv(q, bv):
                for i in range(4):
                    t = 4 * q + i
                    for c in range(DC):
                        P.op("pe", (lambda e, i=i, t=t, c=c: e.matmul(
                            ps[bv][:, i * 128:(i + 1) * 128], lhsT=bufA[:, c, t * 128:(t + 1) * 128], rhs=wv[:, c, :],
                            start=(c == 0), stop=(c == DC - 1))),
                            reads=["wv", ("A", c, q)], writes=[PS(bv)])
                pv4 = ps[bv][:].rearrange("p (t h d) -> p t h d", t=4, h=2)
                d0 = V0[par][0][:, :, 0:64] if q == 0 else Vz[0][:, 4 * q:4 * q + 4, 0:64]
                d1 = V0[par][1][:, :, 64:128] if q == 0 else Vz[1][:, 4 * q:4 * q + 4, 64:128]
                vkey = ("V0", par) if q == 0 else ("V", q)
                P.op("act", (lambda e: e.copy(out=d0, in_=pv4[:, :, 0, :])), reads=[PS(bv)], writes=[vkey])
                P.op("dve", (lambda e: e.tensor_copy(out=d1, in_=pv4[:, :, 1, :])), reads=[PS(bv)], writes=[vkey])

            def items(tg, bank):
                tcs = slice(tg * 512, (tg + 1) * 512)
                out = []

                def mm_pair(w, key, c0):
                    def f():
                        for c in (c0, c0 + 1):
                            P.op("pe", (lambda e, c=c: e.matmul(ps[bank][:], lhsT=w[:, c, :], rhs=bufA[:, c, tcs],
                                                                start=(c == 0), stop=(c == DC - 1))),
                                 reads=[key, ("A", c, tg)], writes=[PS(bank)])
                    return f

                def q_evac():
                    for hf in range(2):
                        rs = slice(64 * hf, 64 * hf + 64)
                        dst = Q0[par][hf][rs, :] if tg == 0 else QTz[hf][rs, tcs]
                        if fox:
                            P.op("dve", (lambda e, rs=rs, dst=dst: e.tensor_scalar(out=dst, in0=ps[bank][rs, :],
                                                                                   scalar1=DH ** -0.5, scalar2=None,
                                                                                   op0=ALU.mult)),
                                 reads=[PS(bank)], writes=[Qk(par, tg)])
                        else:
                            P.op("act", (lambda e, rs=rs, dst=dst: e.mul(out=dst, in_=ps[bank][rs, :], mul=DH ** -0.5)),
                                 reads=[PS(bank)], writes=[Qk(par, tg)])

                def k_evac():
                    kkey = ("KT0", par) if tg == 0 else ("KT", tg)
                    if not fox:
                        dst = K0[par][0] if tg == 0 else KT[:, tcs]
                        P.op("dve", (lambda e: e.tensor_copy(out=dst, in_=ps[bank][:])), reads=[PS(bank)], writes=[kkey])
                    else:
                        for hf in range(2):
                            rs = slice(64 * hf, 64 * hf + 64)
                            dst = K0[par][hf][rs, :] if tg == 0 else KTz[hf][rs, tcs]
                            P.op("dve", (lambda e, rs=rs, dst=dst: e.tensor_copy(out=dst, in_=ps[bank][rs, :])),
                                 reads=[PS(bank)], writes=[kkey])

                def v_tile(i):
                    def f():
                        t = 4 * tg + i
                        for c in range(DC):
                            P.op("pe", (lambda e, c=c: e.matmul(
                                ps[bank][:, i * 128:(i + 1) * 128], lhsT=bufA[:, c, t * 128:(t + 1) * 128],
                                rhs=wv[:, c, :], start=(c == 0), stop=(c == DC - 1))),
                                reads=["wv", ("A", c, tg)], writes=[PS(bank)])
                    return f

                def v_evac():
                    pv4 = ps[bank][:].rearrange("p (t h d) -> p t h d", t=4, h=2)
                    d0 = V0[par][0][:, :, 0:64] if tg == 0 else Vz[0][:, 4 * tg:4 * tg + 4, 0:64]
                    d1 = V0[par][1][:, :, 64:128] if tg == 0 else Vz[1][:, 4 * tg:4 * tg + 4, 64:128]
                    vkey = ("V0", par) if tg == 0 else ("V", tg)
                    P.op("act", (lambda e: e.copy(out=d0, in_=pv4[:, :, 0, :])), reads=[PS(bank)], writes=[vkey])
                    P.op("dve", (lambda e: e.tensor_copy(out=d1, in_=pv4[:, :, 1, :])), reads=[PS(bank)], writes=[vkey])

                out += [mm_pair(wq, "wq", c0) for c0 in (0, 2, 4, 6)] + [q_evac]
                out += [mm_pair(wk, "wk", c0) for c0 in (0, 2, 4, 6)] + [k_evac]
                out += [v_tile(i) for i in range(4)] + [v_evac]
                return out

            return dict(prologue=prologue, aug=aug, gq=gq, gk=gk, gv=gv, items=items)

        def attn_all(npairs):
            steps = []
            for j in range(npairs):
                for qg in range(4):
                    for a in range(4 * qg + 3, -1, -1):
                        for half in range(2):
                            steps.append((j, half, qg, a))
            N = len(steps)
            later, cur = {}, [0]

            def info(n):
                j, half, qg, a = steps[n]
                lo = max(0, 128 * a - 512 * qg)
                amax = 4 * qg + 3
                return dict(j=j, par=j % 2, half=half, qg=qg, a=a, lo=lo, amax=amax, diag=(a >= 4 * qg),
                            base=64 * half, zs=n % (2 if fox else 3), es=n % 5, fs=n % 2, bs=n % 3, ws=n % 3, hg=2 * j + half)

            def kq(I):
                par, half, a, qg, lo = I["par"], I["half"], I["a"], I["qg"], I["lo"]
                return (Kap(par, half, a), Qap(par, half, qg, lo))

            def sb_z(n):
                I = info(n)
                zs, lo = I["zs"], I["lo"]
                kt, qt = kq(I)
                P.op("pe", (lambda e: e.matmul(ps[zs][:, lo:512], lhsT=kt, rhs=qt, start=True, stop=True)),
                     reads=[Kk(I["par"], I["a"]), Qk(I["par"], I["qg"]), "pad"], writes=[PS(zs)])

            def sb_act1(n):
                I = info(n)
                zs, lo, es, fs = I["zs"], I["lo"], I["es"], I["fs"]
                P.op("act", (lambda e: e.activation(out=ef[es][:, lo:512], in_=ps[zs][:, lo:512], func=AF.Exp)),
                     reads=[PS(zs)], writes=[("ef", es)])
                P.op("act", (lambda e: e.activation(out=spf[fs][:, lo:512], in_=ef[es][:, lo:512], func=AF.Ln,
                                                    bias=1.0)),
                     reads=[("ef", es)], writes=[("spf", fs)])

            def sb_dve1(n):
                I = info(n)
                zs, lo, es, fs, bs = I["zs"], I["lo"], I["es"], I["fs"], I["bs"]
                P.op("dve", (lambda e: e.tensor_tensor(out=ef[es][:, lo:512], in0=ps[zs][:, lo:512],
                                                       in1=spf[fs][:, lo:512], op=ALU.subtract)),
                     reads=[PS(zs), ("spf", fs)], writes=[("ef", es)])
                if I["diag"]:
                    P.op("dve", (lambda e: e.tensor_tensor(out=spb[bs][:, lo:lo + 128], in0=spf[fs][:, lo:lo + 128],
                                                           in1=mstrict, op=ALU.mult)),
                         reads=[("spf", fs), "mstrict"], writes=[("spb", bs)])
                    if lo + 128 < 512:
                        P.op("dve", (lambda e: e.tensor_copy(out=spb[bs][:, lo + 128:512],
                                                             in_=spf[fs][:, lo + 128:512])),
                             reads=[("spf", fs)], writes=[("spb", bs)])
                else:
                    P.op("dve", (lambda e: e.tensor_copy(out=spb[bs][:, lo:512], in_=spf[fs][:, lo:512])),
                         reads=[("spf", fs)], writes=[("spb", bs)])

            def sb_g1(n):
                I = info(n)
                lo, bs = I["lo"], I["bs"]
                gb = 3 + I["half"]
                P.op("pe", (lambda e: e.matmul(ps[gb][:, lo:512], lhsT=tri, rhs=spb[bs][:, lo:512],
                                               start=(I["a"] == I["amax"]), stop=False, skip_group_check=True)),
                     reads=["tri", ("spb", bs)], writes=[PS(gb)])

            def sb_d2(n):
                I = info(n)
                lo, es = I["lo"], I["es"]
                gb = 3 + I["half"]
                P.op("dve", (lambda e: e.tensor_tensor(out=ef[es][:, lo:512], in0=ef[es][:, lo:512],
                                                       in1=ps[gb][:, lo:512], op=ALU.subtract)),
                     reads=[("ef", es), PS(gb)], writes=[("ef", es)])

            def sb_g2(n):
                I = info(n)
                lo, bs, a = I["lo"], I["bs"], I["a"]
                gb = 3 + I["half"]
                if a == 0:
                    return
                P.op("pe", (lambda e: e.matmul(ps[gb][:, lo:512], lhsT=triu, rhs=spb[bs][:, lo:512], start=False,
                                               stop=(a == 1), skip_group_check=True)),
                     reads=["triu", ("spb", bs)], writes=[PS(gb)])

            def sb_w(n):
                I = info(n)
                lo, es, ws = I["lo"], I["es"], I["ws"]
                P.op("act", (lambda e: e.activation(out=wb[ws][:, lo:512], in_=ef[es][:, lo:512], func=AF.Exp)),
                     reads=[("ef", es)], writes=[("wb", ws)])

            def sb_mask(n):
                I = info(n)
                lo, ws = I["lo"], I["ws"]
                if I["diag"]:
                    P.op("dve", (lambda e: e.tensor_tensor(out=wb[ws][:, lo:lo + 128], in0=wb[ws][:, lo:lo + 128],
                                                           in1=mstrb, op=ALU.mult)),
                         reads=[("wb", ws), "mstrb"], writes=[("wb", ws)])

            def sb_pv(n):
                I = info(n)
                ws, lo, a, qg, half, j = I["ws"], I["lo"], I["a"], I["qg"], I["half"], I["j"]
                cs = slice(qg * 512, (qg + 1) * 512)
                ob = 5 + qg % 2
                P.op("pe", (lambda e: e.matmul(ps[ob][:, lo:512], lhsT=Vap(I["par"], half, a), rhs=wb[ws][:, lo:512],
                                               start=(a == I["amax"] and half == 0), stop=(a == 0 and half == 1),
                                               skip_group_check=True)),
                     reads=[Vk(I["par"], a), ("wb", ws), "pad"], writes=[PS(ob)])
                if a == 0 and half == 1:
                    later.setdefault(cur[0] + 1, []).append(lambda: P.op(
                        "dve", (lambda e: e.tensor_copy(out=mixT[:, j, cs], in_=ps[ob][:])),
                        reads=[PS(ob)], writes=[("mix", j, qg, 0), ("mix", j, qg, 1)]))

            def fx_z(n):
                I = info(n)
                zs, lo, qg, half = I["zs"], I["lo"], I["qg"], I["half"]
                kt, qt = kq(I)
                P.op("pe", (lambda e: e.matmul(ps[zs][:, lo:512], lhsT=kt, rhs=qt, start=True, stop=not I["diag"],
                                               skip_group_check=True)),
                     reads=[Kk(I["par"], I["a"]), Qk(I["par"], qg),
                            ("QTaug0", I["par"], half) if qg == 0 else ("QTaug", half), "pad"], writes=[PS(zs)])
                if I["diag"]:
                    P.op("pe", (lambda e: e.matmul(ps[zs][:, lo:lo + 128], lhsT=ident[:], rhs=negb, start=False,
                                                   stop=True, skip_group_check=True)),
                         reads=["ident", "negb"], writes=[PS(zs)])

            def fx_w(n):
                I = info(n)
                zs, lo, ws, a, hg = I["zs"], I["lo"], I["ws"], I["a"], I["hg"]
                bias = ncum[:, a, hg:hg + 1]
                P.op("act", (lambda e: e.activation(out=wb[ws][:, lo:512], in_=ps[zs][:, lo:512], func=AF.Exp,
                                                    bias=bias)),
                     reads=[PS(zs), ("ncum", a // 4)], writes=[("wb", ws)])

            def fx_pv(n):
                I = info(n)
                ws, lo, a, qg, half, j = I["ws"], I["lo"], I["a"], I["qg"], I["half"], I["j"]
                cs = slice(qg * 512, (qg + 1) * 512)
                ob = 3 + 2 * half + qg % 2
                P.op("pe", (lambda e: e.matmul(ps[ob][:, lo:512], lhsT=Vap(I["par"], half, a), rhs=wb[ws][:, lo:512],
                                               start=(a == I["amax"]), stop=(a == 0), skip_group_check=True)),
                     reads=[Vk(I["par"], a), ("wb", ws), "pad"], writes=[PS(ob)])
                if a == 0:
                    rr = 64 if half == 0 else 0
                    rs = slice(64 * half, 64 * half + 64)
                    rk, bk, hk = ("r_sb", half), ("bc_sb", half), ("r_hl", half)
                    M = 64 * (half + 1)

                    def t1():
                        P.op("act", (lambda e: e.activation(out=r_sb[half], in_=ps[ob][rr:rr + 1, :], func=AF.Ln)),
                             reads=[PS(ob)], writes=[rk])
                        P.op("act", (lambda e: e.activation(out=r_sb[half], in_=r_sb[half], func=AF.Exp, scale=-1.0)),
                             reads=[rk], writes=[rk])

                    def t2():
                        P.op("dve", (lambda e: e.tensor_copy(out=r_hi[half], in_=r_sb[half])),
                             reads=[rk], writes=[hk])
                        P.op("dve", (lambda e: e.tensor_tensor(out=r_lo[half], in0=r_sb[half], in1=r_hi[half],
                                                               op=ALU.subtract)),
                             reads=[rk, hk], writes=[hk])

                    def t3():
                        P.op("pe", (lambda e: e.matmul(ps[2][0:M, :], lhsT=onesb[rr:rr + 1, 0:M], rhs=r_hi[half],
                                                       start=True, stop=False, skip_group_check=True)),
                             reads=["onesb", hk], writes=[PS(2)])
                        P.op("pe", (lambda e: e.matmul(ps[2][0:M, :], lhsT=onesb[rr:rr + 1, 0:M], rhs=r_lo[half],
                                                       start=False, stop=True, skip_group_check=True)),
                             reads=["onesb", hk], writes=[PS(2)])

                    def t4():
                        P.op("dve", (lambda e: e.tensor_copy(out=bc_sb[half], in_=ps[2][rs, :])),
                             reads=[PS(2)], writes=[bk])

                    def t5():
                        P.op("dve", (lambda e: e.tensor_tensor(out=mixT[rs, j, cs], in0=ps[ob][rs, :], in1=bc_sb[half],
                                                               op=ALU.mult)),
                             reads=[PS(ob), bk], writes=[("mix", j, qg, half)])

                    for dl, fn in ((1, t1), (3, t2), (5, t3), (5, t4), (7, t5)):
                        later.setdefault(cur[0] + dl, []).append(fn)

            if not fox:
                sched = [(sb_z, 0), (sb_act1, 1), (sb_d2, 4), (sb_g1, 3), (sb_dve1, 2), (sb_w, 5), (sb_mask, 6),
                         (sb_pv, 7), (sb_g2, 4)]
            else:
                sched = [(fx_z, 0), (fx_w, 1), (fx_pv, 3)]
            depth = max(k for _, k in sched)
            bg = {}
            PG = [proj_groups(j) for j in range(npairs)]
            PG[0]["prologue"]()
            PG[0]["gq"](0, 0)
            PG[0]["gk"](0, 1)
            PG[0]["gv"](0, 2)
            def spread(fns, first, last):
                n = len(fns)
                for idx, f in enumerate(fns):
                    bg.setdefault(first + (idx * (last - first + 1)) // n, []).append(f)

            for j in range(npairs):
                B = 80 * j
                if fox:
                    bg.setdefault(B, []).append(lambda j=j: PG[j]["aug"](1))
                spread(PG[j]["items"](1, 7), B + 0, B + 7)
                spread(PG[j]["items"](2, 7), B + 8, B + 22)
                spread(PG[j]["items"](3, 7), B + 24, B + 38)
                if j + 1 < npairs:
                    bg.setdefault(B + 50, []).append(PG[j + 1]["prologue"])
                    spread(PG[j + 1]["items"](0, 7), B + 54, B + 68)
            i = 0
            while i < N + depth or later:
                cur[0] = i
                for fn in later.pop(i, []):
                    fn()
                for fn in bg.pop(i, []):
                    fn()
                for fn, k in sched:
                    if 0 <= i - k < N:
                        fn(i - k)
                i += 1

        if cfg.get("npairs", 8) < 8:
            P.op("pool", lambda e: e.memset(arena[:, 0:16384], 0.0),
                 writes=[("mix", j, qg, half) for j in range(8) for qg in range(4) for half in range(2)])
        attn_all(cfg.get("npairs", 8))

        if cfg.get("dbg"):
            MIXK = [("mix", j, qg, half) for j in range(8) for qg in range(4) for half in range(2)]
            dm = nc.dram_tensor("dbg_m", [128, 16384], BF16, kind="ExternalOutput").ap()
            P.dma("sp", lambda e: e.dma_start(out=dm, in_=arena[:, 0:16384]), reads=MIXK, writes=["dbg_m"], lane="dbgm")
            if fox:
                df = nc.dram_tensor("dbg_f", [128, 512], F32, kind="ExternalOutput").ap()
                db = nc.dram_tensor("dbg_b", [16, 4096], BF16, kind="ExternalOutput").ap()
                P.dma("sp", lambda e: e.dma_start(out=df, in_=farena[:, 3456:3456 + 512]),
                      reads=[("nlf", q) for q in range(4)] + [("ncum", q) for q in range(4)], writes=["dbg_f"], lane="dbgf")
                P.dma("sp", lambda e: e.dma_start(out=db, in_=xsf[0:16, 0:4096]),
                      reads=[("nhi", q) for q in range(4)] + [("nlo", q) for q in range(4)], writes=["dbg_b"], lane="dbgb")
                P.wait_all("sp", ["dbg_f", "dbg_b"])
            P.wait_all("sp", ["dbg_m"])
        wo = w_o_b if fox else w_o_a
        wov = wo.rearrange("(c p) n -> p c n", p=128)
        wos = [arena[:, 16384 + i * 4096:16384 + (i + 1) * 4096].rearrange("p (c n) -> p c n", c=DC) for i in range(2)]
        dead = ([("QT0", 0), ("KT0", 0), ("V0", 0), "pad"] + [("QT", q) for q in range(1, 4)]
                + [("KT", q) for q in range(1, 4)] + [("V", q) for q in range(1, 4)]
                + [("QTaug", hf) for hf in range(2)] + [("QTaug0", 0, hf) for hf in range(2)])
        for hf in range(2):
            P.dma("pool", (lambda e, hf=hf: e.dma_start(out=wos[hf], in_=wov[:, :, hf * 512:(hf + 1) * 512])),
                  reads=([("wos", 0)] if hf == 1 else []), writes=[("wos", hf)] + dead, lane="wos%d" % hf)
        xs_alias = [("QT0", 1), ("KT0", 1), ("V0", 1)] + ([("nhi", q) for q in range(4)] + [("nlo", q) for q in range(4)]
                                                         if fox else [])
        n = 0
        fold = cfg.get("nexp", NE) > 0
        for tg in range(4):
            for hf in range(2):
                for i in range(4):
                    t = 4 * tg + i
                    k = n % 4
                    n += 1
                    for c in range(DC):
                        P.op("pe", (lambda e, k=k, c=c, t=t, hf=hf: e.matmul(
                            ps[k][:], lhsT=mixT[:, c, t * 128:(t + 1) * 128], rhs=wos[hf][:, c, :],
                            start=(c == 0), stop=(c == DC - 1))),
                            reads=[("mix", c, t // 4, 0), ("mix", c, t // 4, 1), ("wos", hf)], writes=[PS(k)])
                    hv = h[:, t, hf * 512:(hf + 1) * 512]
                    P.op("dve", (lambda e, k=k, hv=hv: e.tensor_tensor(out=hv, in0=hv, in1=ps[k][:], op=ALU.add)),
                         reads=[PS(k), ("h", t)], writes=[("h", t)])
            if fold:
                if tg >= 1:
                    norm_group_b(tg - 1, 3 + layer, banks=(4, 5, 6, 7))
                norm_group_a(tg, extra_w=xs_alias)
        if fold:
            norm_group_b(3, 3 + layer, banks=(4, 5, 6, 7))

    def moe(layer, nexp, tail_cb=None, head_cb=None):
        P.barrier()
        BGQ = []
        wg = [arena[:, i * 4096:(i + 1) * 4096].rearrange("p (c f) -> p c f", c=DC) for i in range(2)]
        wu = [arena[:, 8192 + i * 4096:8192 + (i + 1) * 4096].rearrange("p (c f) -> p c f", c=DC) for i in range(2)]
        wd = [arena[:, 16384 + i * 4096:16384 + (i + 1) * 4096].rearrange("p (c f) -> p c f", c=4) for i in range(2)]
        hT = [arena[:, 24576 + i * 2048:24576 + (i + 1) * 2048].rearrange("p (c f) -> p c f", c=4) for i in range(2)]
        wr = arena[:, 28672:28672 + 160].rearrange("p (c f) -> p c f", c=DC)
        sg = [farena[:, i * 512:(i + 1) * 512] for i in range(2)]
        rl = farena[:, 1024:1024 + NT * 20].rearrange("p (t f) -> p t f", t=NT)
        comb = farena[:, 1344:1344 + NT * NE].rearrange("p (t f) -> p t f", t=NT)
        o = 1600
        rt = {}
        for nm, w in (("gmax", 1), ("gsh", 4), ("gex", 4), ("gsum", 1), ("gprob", 1), ("gm", 4), ("pen", 4),
                      ("msk", 16), ("m1", 1), ("oh1", 16), ("msk2", 16), ("m2", 1), ("oh2", 16), ("dm", 1),
                      ("ed", 1), ("den", 1), ("w1", 1), ("w2", 1), ("c1", 16)):
            rt[nm] = farena[:, o:o + NT * w].rearrange("p (t f) -> p t f", t=NT)
            o += NT * w
        assert o <= 4608

        if not cfg.get("attn", True):
            norm_transpose(3 + layer)
        wgv = w_group[layer].rearrange("(c p) n -> p c n", p=128)
        wrv = w_router[layer].rearrange("(c p) n -> p c n", p=128)
        wr32 = farena[:, 4608:4768].rearrange("p (c f) -> p c f", c=DC)
        P.dma("sp", (lambda e: e.dma_start(out=wr32[:, :, 0:4], in_=wgv)), writes=["wr32a"], lane="wra")
        P.dma("sp", (lambda e: e.dma_start(out=wr32[:, :, 4:20], in_=wrv)), writes=["wr32b"], lane="wrb")
        P.op("dve", lambda e: e.tensor_copy(out=wr, in_=wr32), reads=["wr32a", "wr32b"], writes=["wr_a", "wr_b"])
        for t in range(NT):
            k = 4 + t % 2
            for c in range(DC):
                P.op("pe", (lambda e, k=k, t=t, c=c: e.matmul(
                    ps[k][:, 0:20], lhsT=bufA[:, c, t * 128:(t + 1) * 128], rhs=wr[:, c, :],
                    start=(c == 0), stop=(c == DC - 1))),
                    reads=[("A", c, t // 4), "wr_a", "wr_b"], writes=[PS(k)])
            P.op("act", (lambda e, k=k, t=t: e.copy(out=rl[:, t, :], in_=ps[k][:, 0:20])),
                 reads=[PS(k)], writes=[("rl", t)])
        RL = [("rl", t) for t in range(NT)]

        def dv(fn, reads, writes):
            P.op("dve", fn, reads=reads, writes=writes)

        def bc(ap, w):
            return ap.to_broadcast([128, NT, w])

        gl = rl[:, :, 0:4]
        el = rl[:, :, 4:20]
        AXX = mybir.AxisListType.X
        dv(lambda e: e.tensor_reduce(out=rt["gmax"], in_=gl, axis=AXX, op=ALU.max), RL, ["gmax"])
        dv(lambda e: e.tensor_tensor(out=rt["gsh"], in0=gl, in1=bc(rt["gmax"], 4), op=ALU.subtract),
           RL + ["gmax"], ["gsh"])
        P.op("act", lambda e: e.activation(out=rt["gex"], in_=rt["gsh"], func=AF.Exp), reads=["gsh"], writes=["gex"])
        dv(lambda e: e.tensor_reduce(out=rt["gsum"], in_=rt["gex"], axis=AXX, op=ALU.add), ["gex"], ["gsum"])
        dv(lambda e: e.reciprocal(out=rt["gprob"], in_=rt["gsum"]), ["gsum"], ["gprob"])
        dv(lambda e: e.tensor_tensor(out=rt["gm"], in0=gl, in1=bc(rt["gmax"], 4), op=ALU.is_equal),
           RL + ["gmax"], ["gm"])
        dv(lambda e: e.tensor_scalar(out=rt["pen"], in0=rt["gm"], scalar1=-1.0, scalar2=1e9,
                                     op0=ALU.add, op1=ALU.mult), ["gm"], ["pen"])
        dv(lambda e: e.tensor_tensor(
            out=rt["msk"].rearrange("p t (g x) -> p t g x", g=4), in0=el.rearrange("p t (g x) -> p t g x", g=4),
            in1=rt["pen"].unsqueeze(3).to_broadcast([128, NT, 4, 4]), op=ALU.add), RL + ["pen"], ["msk"])
        dv(lambda e: e.tensor_reduce(out=rt["m1"], in_=rt["msk"], axis=AXX, op=ALU.max), ["msk"], ["m1"])
        dv(lambda e: e.tensor_tensor(out=rt["oh1"], in0=rt["msk"], in1=bc(rt["m1"], 16), op=ALU.is_equal),
           ["msk", "m1"], ["oh1"])
        dv(lambda e: e.scalar_tensor_tensor(out=rt["msk2"], in0=rt["oh1"], scalar=-1e9, in1=rt["msk"],
                                            op0=ALU.mult, op1=ALU.add), ["oh1", "msk"], ["msk2"])
        dv(lambda e: e.tensor_reduce(out=rt["m2"], in_=rt["msk2"], axis=AXX, op=ALU.max), ["msk2"], ["m2"])
        dv(lambda e: e.tensor_tensor(out=rt["oh2"], in0=rt["msk2"], in1=bc(rt["m2"], 16), op=ALU.is_equal),
           ["msk2", "m2"], ["oh2"])
        dv(lambda e: e.tensor_tensor(out=rt["dm"], in0=rt["m2"], in1=rt["m1"], op=ALU.subtract),
           ["m1", "m2"], ["dm"])
        P.op("act", lambda e: e.activation(out=rt["ed"], in_=rt["dm"], func=AF.Exp), reads=["dm"], writes=["ed"])
        dv(lambda e: e.tensor_scalar(out=rt["den"], in0=rt["ed"], scalar1=1.0, scalar2=None, op0=ALU.add),
           ["ed"], ["den"])
        dv(lambda e: e.reciprocal(out=rt["w1"], in_=rt["den"]), ["den"], ["w1"])
        dv(lambda e: e.tensor_tensor(out=rt["w1"], in0=rt["w1"], in1=rt["gprob"], op=ALU.mult),
           ["w1", "gprob"], ["w1"])
        dv(lambda e: e.tensor_tensor(out=rt["w2"], in0=rt["w1"], in1=rt["ed"], op=ALU.mult),
           ["w1", "ed"], ["w2"])
        dv(lambda e: e.tensor_tensor(out=rt["c1"], in0=rt["oh1"], in1=bc(rt["w1"], 16), op=ALU.mult),
           ["oh1", "w1"], ["c1"])
        dv(lambda e: e.tensor_tensor(out=comb, in0=rt["oh2"], in1=bc(rt["w2"], 16), op=ALU.mult),
           ["oh2", "w2"], ["comb"])
        dv(lambda e: e.tensor_tensor(out=comb, in0=comb, in1=rt["c1"], op=ALU.add), ["comb", "c1"], ["comb"])

        def load_w(ex):
            s = ex % 2
            gv = w_gate[layer, ex].rearrange("(c p) f -> p c f", p=128)
            uv = w_up[layer, ex].rearrange("(c p) f -> p c f", p=128)
            dvw = w_down[layer, ex].rearrange("(c p) f -> p c f", p=128)
            P.dma("pool", (lambda e: e.dma_start(out=wg[s], in_=gv)), writes=[("wg", s)], lane="wg%d" % s)
            P.dma("pool", (lambda e: e.dma_start(out=wu[s], in_=uv)), writes=[("wu", s)], lane="wu%d" % s)
            P.dma("pool", (lambda e: e.dma_start(out=wd[s], in_=dvw)),
                  reads=([("wg", s), ("wu", s)] if ex == 0 else []), writes=[("wd", s)], lane="wd%d" % s)

        def gate_up(ex, tg, hs):
            s = ex % 2
            for fc in range(4):
                kg, ku = fc % 2, 2 + fc % 2
                for c in range(DC):
                    P.op("pe", (lambda e, kg=kg, c=c, fc=fc: e.matmul(
                        ps[kg][:], lhsT=wg[s][:, c, fc * 128:(fc + 1) * 128], rhs=bufA[:, c, tg * 512:(tg + 1) * 512],
                        start=(c == 0), stop=(c == DC - 1))),
                        reads=[("wg", s), ("A", c, tg)], writes=[PS(kg)])
                for c in range(DC):
                    P.op("pe", (lambda e, ku=ku, c=c, fc=fc: e.matmul(
                        ps[ku][:], lhsT=wu[s][:, c, fc * 128:(fc + 1) * 128], rhs=bufA[:, c, tg * 512:(tg + 1) * 512],
                        start=(c == 0), stop=(c == DC - 1))),
                        reads=[("wu", s), ("A", c, tg)], writes=[PS(ku)])
                P.op("act", (lambda e, kg=kg, fc=fc: e.activation(out=sg[fc % 2], in_=ps[kg][:], func=AF.Silu)),
                     reads=[PS(kg)], writes=[("sg", fc % 2)])
                P.op("dve", (lambda e, ku=ku, fc=fc: e.tensor_tensor(
                    out=hT[hs][:, fc, :], in0=sg[fc % 2], in1=ps[ku][:], op=ALU.mult)),
                    reads=[("sg", fc % 2), PS(ku)], writes=[("hT", hs, fc)])
                if BGQ:
                    BGQ.pop(0)()

        def down(ex, tg, hs):
            s = ex % 2
            n = 0
            for i in range(4):
                t = 4 * tg + i
                for half in range(2):
                    k = 4 + n % 3
                    n += 1
                    for fc in range(4):
                        P.op("pe", (lambda e, k=k, fc=fc, i=i, half=half: e.matmul(
                            ps[k][:], lhsT=hT[hs][:, fc, i * 128:(i + 1) * 128],
                            rhs=wd[s][:, fc, half * 512:(half + 1) * 512], start=(fc == 0), stop=(fc == 3))),
                            reads=[("hT", hs, fc), ("wd", s)], writes=[PS(k)])
                    hv = h[:, t, half * 512:(half + 1) * 512]
                    P.op("dve", (lambda e, k=k, hv=hv, t=t: e.scalar_tensor_tensor(
                        out=hv, in0=ps[k][:], scalar=comb[:, t, ex:ex + 1], in1=hv, op0=ALU.mult, op1=ALU.add)),
                        reads=[PS(k), "comb", ("h", t)], writes=[("h", t)])
                    if BGQ:
                        BGQ.pop(0)()

        steps = [(ex, tg) for ex in range(nexp) for tg in range(4)]
        load_w(0)
        if head_cb is not None:
            head_cb()
        prev = None
        for n, (ex, tg) in enumerate(steps):
            gate_up(ex, tg, n % 2)
            if prev is not None:
                down(prev[0], prev[1], (n - 1) % 2)
                if tail_cb is not None and prev[0] == nexp - 1:
                    tail_cb(prev[1], BGQ)
            if tg == 0 and ex + 1 < nexp:
                load_w(ex + 1)
            prev = (ex, tg)
        down(prev[0], prev[1], (len(steps) - 1) % 2)
        if tail_cb is not None:
            tail_cb(prev[1], BGQ)
        while BGQ:
            BGQ.pop(0)()

    xs32 = xs[:].rearrange("p s a d -> p (s a d)").bitcast(F32)
    gfin = xs32[:, 0:D]
    ost = [xs32[:, (1 + i) * D:(2 + i) * D] for i in range(2)]
    ov = out.rearrange("(t p) d -> p t d", p=128)

    def final_setup():
        P.dma("sp", lambda e: e.dma_start(out=gfin, in_=final_norm.partition_broadcast(128)),
              writes=["gfin"] + [("xs", sl, i) for sl in range(2) for i in range(4)], lane="gfin")

    def final_group(tg, bgq=None):
        norm_stats(4 * tg, 4 * tg + 4)
        for i in range(4):
            t = 4 * tg + i
            s = t % 2
            P.op("dve", (lambda e, t=t, s=s: e.scalar_tensor_tensor(
                out=ost[s], in0=h[:, t, :], scalar=rstd[:, t:t + 1], in1=gfin, op0=ALU.mult, op1=ALU.mult)),
                reads=[("h", t), ("rstd", t), "gfin"], writes=[("ost", s)])
            P.dma("sp", (lambda e, t=t, s=s: e.dma_start(out=ov[:, t, :], in_=ost[s])),
                  reads=[("ost", s)], writes=[("out", t)], lane="o%d" % s)

    def fox_norm_cb(tg, bgq):
        while bgq:
            bgq.pop(0)()
        norm_group_a(tg)
        for c in range(DC):
            bgq.append(lambda c=c: norm_group_b(tg, None, banks=(7,), chunks=[c]))

    layers = cfg["layers"]
    nexp = cfg.get("nexp", NE)
    for li, layer in enumerate(layers):
        last = (li == len(layers) - 1)
        if cfg.get("attn", True):
            attention(layer, prenormed=(li > 0 and nexp > 0 and layer == 1))
        if nexp > 0:
            if last:
                moe(layer, nexp, tail_cb=final_group, head_cb=final_setup)
            elif cfg.get("attn", True) and layers[li + 1] == 1:
                moe(layer, nexp, tail_cb=fox_norm_cb)
            else:
                moe(layer, nexp)
    if nexp == 0:
        P.barrier()
        final_setup()
        for tg in range(4):
            final_group(tg)
    P.wait_all("sp", [("out", t) for t in range(NT)])
    P.emit(nc)
    es.close()
    return nc


_CACHE = {}


def _prep(inputs):
    f = lambda a: np.ascontiguousarray(np.asarray(a, dtype=np.float32))
    shared = {
        "attn_norm": f(inputs["attn_norm"]),
        "w_qkv_a": f(inputs["w_qkv_a"])[0],
        "w_o_a": f(inputs["w_o_a"])[0],
        "kv_norm": f(inputs["kv_norm"]).reshape(1, D),
        "w_kvf": f(inputs["w_kvf"]),
        "b_f": f(inputs["b_f"]).reshape(1, H),
        "w_q_b": f(inputs["w_q_b"])[0],
        "w_o_b": f(inputs["w_o_b"])[0],
        "moe_norm": f(inputs["moe_norm"]),
        "w_group": f(inputs["w_group"]),
        "w_router": f(inputs["w_router"]),
        "w_gate": f(inputs["w_gate"]),
        "w_up": f(inputs["w_up"]),
        "w_down": f(inputs["w_down"]),
        "final_norm": f(inputs["final_norm"]).reshape(1, D),
    }
    xin = f(inputs["x"])
    return [dict(shared, x=xin[b]) for b in range(xin.shape[0])]


def run_cfg(inputs, cfg, trace=False):
    nc = bass.Bass("TRN2", target_bir_lowering=False)
    build(nc, cfg)
    in_maps = _prep(inputs)
    res = run_bass_kernel_spmd(nc, in_maps, core_ids=list(range(len(in_maps))), trace=trace)
    outs = np.stack([np.asarray(r["out"]) for r in res.results], axis=0)
    if cfg.get("dbg"):
        res.dbg = {k: np.asarray(v) for k, v in res.results[0].items() if k.startswith("dbg")}
    return outs, res


def kernel(**inputs):
    outs, _ = run_cfg(inputs, dict(layers=[0, 1], attn=True, nexp=NE))
    return outs.astype(np.float32)
```

```python
from contextlib import ExitStack

import numpy as np
import concourse.bass as bass
import concourse.mybir as mybir
from concourse.bass_utils import run_bass_kernel_spmd

F32 = mybir.dt.float32
BF16 = mybir.dt.bfloat16
AF = mybir.ActivationFunctionType
ALU = mybir.AluOpType

D = 1024
S = 2048
NT = S // 128
DC = D // 128
H = 16
DH = 64
NE = 16
FE = 512
EPS = 1e-6
NEG = -30000.0
ARENA = 34688
FARENA = 5760
ENGS = ("pe", "act", "dve", "pool", "sp")


class Prog:
    def __init__(self):
        self.ops = []

    def op(self, eng, fn, reads=(), writes=()):
        self.ops.append(dict(eng=eng, fn=fn, r=tuple(reads), w=tuple(writes), kind="op", prod=eng))

    def dma(self, q, fn, reads=(), writes=(), lane=None):
        assert lane is not None
        self.ops.append(dict(eng=q, fn=fn, r=tuple(reads), w=tuple(writes), kind="dma", prod="L:" + lane))

    def barrier(self):
        for e in ENGS:
            self.ops.append(dict(eng=e, fn=None, r=(), w=(), kind="bar", prod=None))

    def wait_all(self, eng, reads):
        self.ops.append(dict(eng=eng, fn=None, r=tuple(reads), w=(), kind="wait", prod=None))

    def schedule(self):
        ops = self.ops
        last_w, readers = {}, {}
        seq, cnt = [], {}
        waited = {e: {} for e in ENGS}
        last_in = {}
        signal = set()
        for i, o in enumerate(ops):
            p = o["prod"]
            if p is not None:
                cnt[p] = cnt.get(p, 0) + 1
                seq.append(cnt[p])
            else:
                seq.append(None)
            ce = o["eng"]
            raw = set()
            if o["kind"] == "bar":
                raw = set(last_in.values())
            else:
                for k in o["r"]:
                    if k in last_w:
                        raw.add(last_w[k])
                for k in o["w"]:
                    if k in last_w:
                        raw.add(last_w[k])
                    raw.update(readers.get(k, ()))
            need = {}
            for j in raw:
                pj = ops[j]["prod"]
                if pj == "pe" and ce == "pe" and o["kind"] == "op":
                    continue
                if waited[ce].get(pj, 0) >= seq[j]:
                    continue
                if pj not in need or seq[j] > seq[need[pj]]:
                    need[pj] = j
            o["waits"] = []
            for pj, j in need.items():
                o["waits"].append(j)
                waited[ce][pj] = seq[j]
                signal.add(j)
            for k in o["w"]:
                last_w[k] = i
                readers[k] = []
            for k in o["r"]:
                readers.setdefault(k, []).append(i)
            if p is not None:
                last_in[p] = i
        val, run = {}, {}
        for i, o in enumerate(ops):
            p = o["prod"]
            if p is None:
                continue
            if i in signal:
                run[p] = run.get(p, 0) + (16 if o["kind"] == "dma" else 1)
            val[i] = run.get(p, 0)
        self.signal, self.val = signal, val
        self.prods = sorted(cnt.keys())

    def emit(self, nc):
        self.schedule()
        ops = self.ops
        with ExitStack() as es:
            sems = {}
            for p in self.prods:
                sems[p] = es.enter_context(nc.semaphore("s_" + p.replace(":", "_")))
            block = es.enter_context(nc.Block())

            def run(engname, e):
                for i, o in enumerate(ops):
                    if o["eng"] != engname:
                        continue
                    for j in o["waits"]:
                        e.wait_ge(sems[ops[j]["prod"]], self.val[j])
                    if o["fn"] is not None:
                        ins = o["fn"](e)
                        if i in self.signal:
                            ins.then_inc(sems[o["prod"]], 16 if o["kind"] == "dma" else 1)

            @block.tensor
            def _(e):
                run("pe", e)

            @block.scalar
            def _(e):
                run("act", e)

            @block.vector
            def _(e):
                run("dve", e)

            @block.gpsimd
            def _(e):
                run("pool", e)

            @block.sync
            def _(e):
                run("sp", e)


def build(nc, cfg):
    P = Prog()
    es = ExitStack()
    dr = {}

    def din(name, shape):
        dr[name] = nc.dram_tensor(name, list(shape), F32, kind="ExternalInput").ap()
        return dr[name]

    x = din("x", (S, D))
    attn_norm = din("attn_norm", (2, D))
    w_qkv_a = din("w_qkv_a", (D, 3 * D))
    w_o_a = din("w_o_a", (D, D))
    kv_norm = din("kv_norm", (1, D))
    w_kvf = din("w_kvf", (D, 2 * D + H))
    b_f = din("b_f", (1, H))
    w_q_b = din("w_q_b", (D, D))
    w_o_b = din("w_o_b", (D, D))
    moe_norm = din("moe_norm", (2, D))
    w_group = din("w_group", (2, D, 4))
    w_router = din("w_router", (2, D, NE))
    w_gate = din("w_gate", (2, NE, D, FE))
    w_up = din("w_up", (2, NE, D, FE))
    w_down = din("w_down", (2, NE, FE, D))
    final_norm = din("final_norm", (1, D))
    out = nc.dram_tensor("out", [S, D], F32, kind="ExternalOutput").ap()

    def sb(name, shape, dt):
        return es.enter_context(nc.sbuf_tensor(name, list(shape), dt))

    h = sb("h", (128, NT, D), F32)
    bufA = sb("bufA", (128, DC, S), BF16)
    arena = sb("arena", (128, ARENA), BF16)
    farena = sb("farena", (128, FARENA), F32)
    xs = sb("xs", (128, 2, 4, D), BF16)
    sqj = sb("sqj", (128, D), BF16)
    ss = sb("ss", (128, NT), F32)
    lnv = sb("lnv", (128, NT), F32)
    rstd = sb("rstd", (128, NT), F32)
    gcols = sb("gcols", (128, 5, DC), F32)
    ident = sb("ident", (128, 128), BF16)
    onesb = sb("onesb", (128, 128), BF16)
    onesf = sb("onesf", (128, 128), F32)
    ps = [es.enter_context(nc.psum_tensor("ps%d" % k, [128, 512], F32)) for k in range(8)]

    def PS(k):
        return ("ps", k)

    P.op("pool", lambda e: e.memset(onesb[:], 1.0), writes=["onesb"])
    P.op("pool", lambda e: e.memset(onesf[:], 1.0), writes=["onesf"])
    P.op("pool", lambda e: e.affine_select(out=ident[:], in_=onesb[:], pattern=[[-1, 128]],
                                            compare_op=ALU.is_equal, fill=0.0, base=0, channel_multiplier=1),
         reads=["onesb"], writes=["ident"])
    gsrc = [attn_norm[0:1, :], attn_norm[1:2, :], kv_norm[0:1, :], moe_norm[0:1, :], moe_norm[1:2, :]]
    for n, g in enumerate(gsrc):
        P.dma("act", (lambda e, n=n, g=g: e.dma_start(
            out=gcols[:, n, :], in_=g.rearrange("o (c p) -> p (o c)", p=128), allow_slow_non_contiguous=True)),
            writes=[("gcols", n)], lane="g%d" % n)
    xv = x.rearrange("(t p) d -> p t d", p=128)
    for q in range(4):
        P.dma("sp", (lambda e, q=q: e.dma_start(out=h[:, 4 * q:4 * q + 4, :], in_=xv[:, 4 * q:4 * q + 4, :])),
              reads=([("h", 0)] if q == 1 else []),
              writes=[("h", t) for t in range(4 * q, 4 * q + 4)], lane="x%d" % q)

    def norm_stats(t0, t1):
        for t in range(t0, t1):
            P.op("act", (lambda e, t=t: e.activation(out=sqj[:], in_=h[:, t, :], func=AF.Square,
                                                      accum_out=ss[:, t:t + 1])),
                 reads=[("h", t)], writes=["sqj", ("ss", t)])
        P.op("act", (lambda e: e.activation(out=lnv[:, t0:t1], in_=ss[:, t0:t1], func=AF.Ln,
                                            bias=EPS, scale=1.0 / D)),
             reads=[("ss", t) for t in range(t0, t1)], writes=[("lnv", t) for t in range(t0, t1)])
        P.op("act", (lambda e: e.activation(out=rstd[:, t0:t1], in_=lnv[:, t0:t1], func=AF.Exp, scale=-0.5)),
             reads=[("lnv", t) for t in range(t0, t1)], writes=[("rstd", t) for t in range(t0, t1)])

    def norm_group_a(tg, extra_w=()):
        sl = tg % 2
        norm_stats(4 * tg, 4 * tg + 4)
        for i in range(4):
            t = 4 * tg + i
            P.op("dve", (lambda e, t=t, i=i: e.tensor_scalar(
                out=xs[:, sl, i, :], in0=h[:, t, :], scalar1=rstd[:, t:t + 1], scalar2=None, op0=ALU.mult)),
                reads=[("h", t), ("rstd", t)], writes=[("xs", sl, i)] + list(extra_w))

    def norm_group_b(tg, gain_idx, banks=(0, 1, 2, 3), chunks=range(DC)):
        sl = tg % 2
        for c in chunks:
            k = banks[c % len(banks)]
            for i in range(4):
                P.op("pe", (lambda e, k=k, i=i, c=c: e.matmul(
                    ps[k][:, i * 128:(i + 1) * 128], lhsT=xs[:, sl, i, c * 128:(c + 1) * 128],
                    rhs=ident[:], start=True, stop=True)),
                    reads=[("xs", sl, i), "ident"], writes=[PS(k)])
            dst = bufA[:, c, tg * 512:(tg + 1) * 512]
            if gain_idx is None:
                if c % 2 == 0:
                    P.op("act", (lambda e, k=k, dst=dst: e.copy(out=dst, in_=ps[k][:])),
                         reads=[PS(k)], writes=[("A", c, tg)])
                else:
                    P.op("dve", (lambda e, k=k, dst=dst: e.tensor_copy(out=dst, in_=ps[k][:])),
                         reads=[PS(k)], writes=[("A", c, tg)])
            else:
                gc = gcols[:, gain_idx, c:c + 1]
                if c % 2 == 0:
                    P.op("act", (lambda e, k=k, dst=dst, gc=gc: e.activation(
                        out=dst, in_=ps[k][:], func=AF.Copy, scale=gc)),
                        reads=[PS(k), ("gcols", gain_idx)], writes=[("A", c, tg)])
                else:
                    P.op("dve", (lambda e, k=k, dst=dst, gc=gc: e.tensor_scalar(
                        out=dst, in0=ps[k][:], scalar1=gc, scalar2=None, op0=ALU.mult)),
                        reads=[PS(k), ("gcols", gain_idx)], writes=[("A", c, tg)])

    def norm_group(tg, gain_idx, banks=(0, 1, 2, 3), extra_w=()):
        norm_group_a(tg, extra_w)
        norm_group_b(tg, gain_idx, banks)

    def norm_transpose(gain_idx):
        for tg in range(4):
            norm_group(tg, gain_idx)

    A_ALL = [("A", c, tg) for c in range(DC) for tg in range(4)]


    def attention(layer, prenormed=False):
        fox = (layer == 1)
        P.barrier()
        mixT = arena[:, 0:16384].rearrange("p (c t) -> p c t", c=DC)
        o = 16384
        QTz = [arena[:, o + i * 2048:o + (i + 1) * 2048] for i in range(2)]
        o += 4096
        if not fox:
            KT = arena[:, o:o + 2048]
            KTz = [KT, KT]
            o += 2048
        else:
            KTz = [arena[:, o + i * 2048:o + (i + 1) * 2048] for i in range(2)]
            o += 4096
        Vz = [arena[:, o + i * 2048:o + (i + 1) * 2048].rearrange("p (t d) -> p t d", t=NT) for i in range(2)]
        o += 4096
        wq = arena[:, o:o + 1024].rearrange("p (c n) -> p c n", c=DC)
        wk = arena[:, o + 1024:o + 2048].rearrange("p (c n) -> p c n", c=DC)
        wv = arena[:, o + 2048:o + 3072].rearrange("p (c n) -> p c n", c=DC)
        o += 3072
        wb = [arena[:, o + i * 512:o + (i + 1) * 512] for i in range(3)]
        o += 1536
        tri = arena[:, o:o + 128]
        o += 128
        xsf = xs[:].rearrange("p s a d -> p (s a d)")
        if not fox:
            triu = arena[:, o:o + 128]
            mstrb = arena[:, o + 128:o + 256]
            o += 256
            spb = [arena[:, o + i * 512:o + (i + 1) * 512] for i in range(3)]
            o += 1536
        else:
            nhi = xsf[0:16, 0:2048]
            nlo = xsf[0:16, 2048:4096]
            wfb = arena[:, o:o + 128].rearrange("p (c n) -> p c n", c=DC)
            negb = arena[:, o + 128:o + 256]
            o += 256
            r_hi = [arena[64:65, o:o + 512], arena[0:1, o:o + 512]]
            r_lo = [arena[64:65, o + 512:o + 1024], arena[0:1, o + 512:o + 1024]]
            o += 1024
        assert o <= ARENA
        Q0 = [[QTz[hf][:, 0:512] for hf in range(2)], [xsf[:, 4096 + hf * 512:4096 + (hf + 1) * 512] for hf in range(2)]]
        if not fox:
            K0 = [[KT[:, 0:512]] * 2, [xsf[:, 5120:5632]] * 2]
        else:
            K0 = [[KTz[hf][:, 0:512] for hf in range(2)], [xsf[:, 5120 + hf * 512:5120 + (hf + 1) * 512] for hf in range(2)]]
        V0 = [[Vz[hf][:, 0:4, :] for hf in range(2)],
              [xsf[:, 6144 + hf * 512:6144 + (hf + 1) * 512].rearrange("p (t d) -> p t d", t=4) for hf in range(2)]]

        def Qap(par, hf, qg, lo):
            return Q0[par][hf][:, lo:512] if qg == 0 else QTz[hf][:, qg * 512 + lo:(qg + 1) * 512]

        def Kap(par, hf, a):
            return K0[par][hf][:, a * 128:(a + 1) * 128] if a < 4 else KTz[hf][:, a * 128:(a + 1) * 128]

        def Vap(par, hf, a):
            return V0[par][hf][:, a, :] if a < 4 else Vz[hf][:, a, :]

        def Qk(par, qg):
            return ("QT0", par) if qg == 0 else ("QT", qg)

        def Kk(par, a):
            return ("KT0", par) if a < 4 else ("KT", a // 4)

        def Vk(par, a):
            return ("V0", par) if a < 4 else ("V", a // 4)
        stg = [farena[:, i * 1024:(i + 1) * 1024].rearrange("p (c n) -> p c n", c=DC) for i in range(2)]
        fo = 2048
        if not fox:
            ef = [farena[:, fo + i * 512:fo + (i + 1) * 512] for i in range(5)]
            spf = [farena[:, fo + 2560 + i * 512:fo + 2560 + (i + 1) * 512] for i in range(2)]
            fo += 3584
            mstrict = farena[:, fo:fo + 128]
            fo += 128
        else:
            uc = farena[:, fo:fo + 1408]
            fo += 1408
            nlf = farena[:, fo:fo + 256].rearrange("p (t h) -> p t h", t=NT)
            ncum = farena[:, fo + 256:fo + 512].rearrange("p (t h) -> p t h", t=NT)
            fo += 512
            bfb = farena[:, fo:fo + 16]
            fo += 16
            spre = farena[:, fo:fo + 256].rearrange("p (t h) -> p t h", t=NT)
            fo += 256
            r_sb = [farena[64:65, fo:fo + 512], farena[0:1, fo:fo + 512]]
            bc_sb = [farena[0:64, fo + 512:fo + 1024], farena[64:128, fo + 512:fo + 1024]]
            fo += 1024
            negm2 = farena[:, fo:fo + 128]
            fo += 128
            stf = farena[:, fo:fo + 128].rearrange("p (c n) -> p c n", c=DC)
            fo += 128
        assert fo <= FARENA

        if not prenormed:
            norm_transpose(None)

        P.op("pool", lambda e: e.affine_select(out=tri, in_=onesb[:], pattern=[[-1, 128]], compare_op=ALU.is_gt,
                                                fill=0.0, base=0, channel_multiplier=1),
             reads=["onesb"], writes=["tri"])
        if not fox:
            P.op("pool", lambda e: e.affine_select(out=mstrict, in_=onesf[:], pattern=[[1, 128]],
                                                    compare_op=ALU.is_gt, fill=0.0, base=0, channel_multiplier=-1),
                 reads=["onesf"], writes=["mstrict"])
            P.op("pool", lambda e: e.tensor_copy(out=mstrb, in_=mstrict), reads=["mstrict"], writes=["mstrb"])
            P.op("pool", lambda e: e.affine_select(out=triu, in_=onesb[:], pattern=[[1, 128]], compare_op=ALU.is_ge,
                                                    fill=0.0, base=0, channel_multiplier=-1),
                 reads=["onesb"], writes=["triu"])
        else:
            P.op("pool", lambda e: e.affine_select(out=negm2, in_=onesf[:], pattern=[[1, 128]],
                                                    compare_op=ALU.is_ge, fill=0.0, base=0, channel_multiplier=-1),
                 reads=["onesf"], writes=["negm2"])
            P.op("pool", lambda e: e.tensor_scalar(out=negm2, in0=negm2, scalar1=-1.0, scalar2=-NEG,
                                                   op0=ALU.add, op1=ALU.mult),
                 reads=["negm2"], writes=["negm2"])
            P.op("pool", lambda e: e.tensor_copy(out=negb, in_=negm2), reads=["negm2"], writes=["negb"])
            P.op("pool", lambda e: e.memset(uc, 1.0), writes=["uc"])
            P.op("pool", lambda e: e.affine_select(out=uc, in_=uc, pattern=[[1, 1408]], compare_op=ALU.is_ge,
                                                    fill=0.0, base=-384, channel_multiplier=-1),
                 reads=["uc"], writes=["uc"])
            P.dma("sp", lambda e: e.dma_start(out=bfb, in_=b_f.partition_broadcast(128)), writes=["bfb"], lane="bfb")

        P.op("dve", lambda e: e.memset(QTz[0][64:128, :], 0.0), writes=["pad"])
        P.op("dve", lambda e: e.memset(QTz[1][0:64, :], 0.0), writes=["pad"])
        P.op("dve", lambda e: e.memset(Vz[0][:, :, 64:128], 0.0), writes=["pad"])
        P.op("dve", lambda e: e.memset(Vz[1][:, :, 0:64], 0.0), writes=["pad"])
        if fox:
            P.op("dve", lambda e: e.memset(KTz[0][64:128, :], 0.0), writes=["pad"])
            P.op("dve", lambda e: e.memset(KTz[1][0:64, :], 0.0), writes=["pad"])
            P.op("dve", lambda e: e.memset(KTz[0][64:66, :], 1.0), writes=["pad"])
            P.op("dve", lambda e: e.memset(KTz[1][0:2, :], 1.0), writes=["pad"])
            P.op("dve", lambda e: e.memset(Vz[0][:, :, 64:65], 1.0), writes=["pad"])
            P.op("dve", lambda e: e.memset(Vz[1][:, :, 0:1], 1.0), writes=["pad"])

        XS1 = [("xs", 1, i) for i in range(4)]
        P.op("dve", lambda e: e.memset(Q0[1][0][64:128, :], 0.0), writes=["pad"] + XS1)
        P.op("dve", lambda e: e.memset(Q0[1][1][0:64, :], 0.0), writes=["pad"] + XS1)
        P.op("dve", lambda e: e.memset(V0[1][0][:, :, 64:128], 0.0), writes=["pad"] + XS1)
        P.op("dve", lambda e: e.memset(V0[1][1][:, :, 0:64], 0.0), writes=["pad"] + XS1)
        if fox:
            P.op("dve", lambda e: e.memset(K0[1][0][64:128, :], 0.0), writes=["pad"] + XS1)
            P.op("dve", lambda e: e.memset(K0[1][1][0:64, :], 0.0), writes=["pad"] + XS1)
            P.op("dve", lambda e: e.memset(K0[1][0][64:66, :], 1.0), writes=["pad"] + XS1)
            P.op("dve", lambda e: e.memset(K0[1][1][0:2, :], 1.0), writes=["pad"] + XS1)
            P.op("dve", lambda e: e.memset(V0[1][0][:, :, 64:65], 1.0), writes=["pad"] + XS1)
            P.op("dve", lambda e: e.memset(V0[1][1][:, :, 0:1], 1.0), writes=["pad"] + XS1)

        def load_fold(src_ap, gi, dst, sl, width, key):
            sv = src_ap.rearrange("(c p) n -> p c n", p=128)
            st = stg[sl][:, :, 0:width] if width == 128 else stf
            skey = ("stg", sl) if width == 128 else "stf"
            P.dma("sp", (lambda e: e.dma_start(out=st, in_=sv)), writes=[skey], lane="stg%d" % sl if width == 128 else "stf")
            for c in range(DC):
                P.op("pool", (lambda e, c=c: e.tensor_scalar(out=dst[:, c, :], in0=st[:, c, :],
                                                             scalar1=gcols[:, gi, c:c + 1], scalar2=1.0,
                                                             op0=ALU.mult, op1=ALU.mult)),
                     reads=[skey, ("gcols", gi)], writes=[key])

        if fox:
            load_fold(w_kvf[:, 2 * D:2 * D + H], 2, wfb, 0, 16, "wfb")
            for q in range(4):
                k = q % 2
                for i in range(4):
                    t = 4 * q + i
                    for c in range(DC):
                        P.op("pe", (lambda e, k=k, i=i, t=t, c=c: e.matmul(
                            ps[k][:, i * 16:(i + 1) * 16], lhsT=bufA[:, c, t * 128:(t + 1) * 128], rhs=wfb[:, c, :],
                            start=(c == 0), stop=(c == DC - 1))),
                            reads=[("A", c, q), "wfb"], writes=[PS(k)])
                P.op("dve", (lambda e, k=k, q=q: e.tensor_tensor(
                    out=nlf[:, 4 * q:4 * q + 4, :], in0=ps[k][:, 0:64].rearrange("p (t h) -> p t h", t=4),
                    in1=bfb.unsqueeze(1).to_broadcast([128, 4, 16]), op=ALU.add)),
                    reads=[PS(k), "bfb"], writes=[("nlf", q)])
            NLF = [("nlf", q) for q in range(4)]
            nlf2 = farena[:, 2048 + 1408:2048 + 1408 + 256]
            P.op("act", lambda e: e.activation(out=nlf2, in_=nlf2, func=AF.Exp, scale=-1.0), reads=NLF, writes=NLF)
            P.op("act", lambda e: e.activation(out=nlf2, in_=nlf2, func=AF.Ln, bias=1.0), reads=NLF, writes=NLF)
            P.op("dve", lambda e: e.memset(spre[:, 0, :], 0.0), writes=[("spre", 0)])
            for t in range(1, NT):
                P.op("dve", (lambda e, t=t: e.tensor_tensor(out=spre[:, t, :], in0=spre[:, t - 1, :],
                                                            in1=nlf[:, t - 1, :], op=ALU.add)),
                     reads=NLF + [("spre", t - 1)], writes=[("spre", t)])
            SPRE = [("spre", t) for t in range(NT)]
            for tg in range(4):
                P.op("pe", (lambda e, tg=tg: e.matmul(ps[7][0:16, :], lhsT=spre[:, 4 * tg, :], rhs=uc[:, 896:1408],
                                                      start=True, stop=False)),
                     reads=SPRE + ["uc"], writes=[PS(7)])
                for dl in range(4):
                    off = 384 - 128 * dl
                    P.op("pe", (lambda e, tg=tg, dl=dl, off=off: e.matmul(
                        ps[7][0:16, :], lhsT=nlf[:, 4 * tg + dl, :], rhs=uc[:, off:off + 512],
                        start=False, stop=(dl == 3))),
                        reads=NLF + ["uc"], writes=[PS(7)])
                cs = slice(tg * 512, (tg + 1) * 512)
                P.op("dve", (lambda e, cs=cs: e.tensor_scalar(out=nhi[:, cs], in0=ps[7][0:16, :], scalar1=-1.0,
                                                              scalar2=None, op0=ALU.mult)),
                     reads=[PS(7)], writes=[("nhi", tg)])
                P.op("dve", (lambda e, cs=cs: e.scalar_tensor_tensor(out=nlo[:, cs], in0=ps[7][0:16, :], scalar=-1.0,
                                                                     in1=nhi[:, cs], op0=ALU.mult, op1=ALU.subtract)),
                     reads=[PS(7), ("nhi", tg)], writes=[("nlo", tg)])
            for q in range(4):
                k = 4 + q % 2
                for i in range(4):
                    t = 4 * q + i
                    P.op("pe", (lambda e, k=k, i=i, t=t: e.matmul(
                        ps[k][:, i * 16:(i + 1) * 16], lhsT=onesf[:], rhs=spre[:, t, :], start=True, stop=False)),
                        reads=SPRE + ["onesf"], writes=[PS(k)])
                    P.op("pe", (lambda e, k=k, i=i, t=t: e.matmul(
                        ps[k][:, i * 16:(i + 1) * 16], lhsT=uc[:, 384:512], rhs=nlf[:, t, :], start=False, stop=True)),
                        reads=NLF + ["uc"], writes=[PS(k)])
                P.op("act", (lambda e, k=k, q=q: e.copy(out=ncum[:, 4 * q:4 * q + 4, :],
                                                        in_=ps[k][:, 0:64].rearrange("p (t h) -> p t h", t=4))),
                     reads=[PS(k)], writes=[("ncum", q)])

        def proj_groups(j):
            par = j % 2
            cs = slice(128 * j, 128 * j + 128)
            if not fox:
                srcs = [(w_qkv_a[:, cs], 0), (w_qkv_a[:, D + 128 * j:D + 128 * j + 128], 0),
                        (w_qkv_a[:, 2 * D + 128 * j:2 * D + 128 * j + 128], 0)]
            else:
                srcs = [(w_q_b[:, cs], 1), (w_kvf[:, cs], 2), (w_kvf[:, D + 128 * j:D + 128 * j + 128], 2)]

            def aug(part):
                for hf in range(2):
                    hg = 2 * j + hf
                    r0 = 64 if hf == 0 else 0
                    for r, srcrow, ln in ((r0, nhi, "a"), (r0 + 1, nlo, "b")):
                        if part == 0:
                            dst, sc, key = Q0[par][hf][r:r + 1, :], slice(0, 512), ("QTaug0", par, hf)
                        else:
                            dst, sc, key = QTz[hf][r:r + 1, 512:2048], slice(512, 2048), ("QTaug", hf)
                        P.dma("sp", (lambda e, dst=dst, sc=sc, hg=hg, srcrow=srcrow: e.dma_start(
                            out=dst, in_=srcrow[hg:hg + 1, sc])),
                            reads=[("nhi", q) for q in range(4)] + [("nlo", q) for q in range(4)], writes=[key],
                            lane="aug%d%s%d" % (hf, ln, part))

            def prologue():
                load_fold(srcs[0][0], srcs[0][1], wq, 0, 128, "wq")
                load_fold(srcs[1][0], srcs[1][1], wk, 1, 128, "wk")
                load_fold(srcs[2][0], srcs[2][1], wv, 0, 128, "wv")
                if fox:
                    aug(0)

            def gq(tg, bq):
                tcs = slice(tg * 512, (tg + 1) * 512)
                for c in range(DC):
                    P.op("pe", (lambda e, c=c: e.matmul(ps[bq][:], lhsT=wq[:, c, :], rhs=bufA[:, c, tcs],
                                                        start=(c == 0), stop=(c == DC - 1))),
                         reads=["wq", ("A", c, tg)], writes=[PS(bq)])
                for hf in range(2):
                    rs = slice(64 * hf, 64 * hf + 64)
                    dst = Q0[par][hf][rs, :] if tg == 0 else QTz[hf][rs, tcs]
                    if fox:
                        P.op("dve", (lambda e, rs=rs, dst=dst: e.tensor_scalar(out=dst, in0=ps[bq][rs, :],
                                                                               scalar1=DH ** -0.5, scalar2=None,
                                                                               op0=ALU.mult)),
                             reads=[PS(bq)], writes=[Qk(par, tg)])
                    else:
                        P.op("act", (lambda e, rs=rs, dst=dst: e.mul(out=dst, in_=ps[bq][rs, :], mul=DH ** -0.5)),
                             reads=[PS(bq)], writes=[Qk(par, tg)])

            def gk(tg, bk):
                tcs = slice(tg * 512, (tg + 1) * 512)
                for c in range(DC):
                    P.op("pe", (lambda e, c=c: e.matmul(ps[bk][:], lhsT=wk[:, c, :], rhs=bufA[:, c, tcs],
                                                        start=(c == 0), stop=(c == DC - 1))),
                         reads=["wk", ("A", c, tg)], writes=[PS(bk)])
                kkey = ("KT0", par) if tg == 0 else ("KT", tg)
                if not fox:
                    dst = K0[par][0] if tg == 0 else KT[:, tcs]
                    P.op("dve", (lambda e: e.tensor_copy(out=dst, in_=ps[bk][:])), reads=[PS(bk)], writes=[kkey])
                else:
                    for hf in range(2):
                        rs = slice(64 * hf, 64 * hf + 64)
                        dst = K0[par][hf][rs, :] if tg == 0 else KTz[hf][rs, tcs]
                        P.op("dve", (lambda e, rs=rs, dst=dst: e.tensor_copy(out=dst, in_=ps[bk][rs, :])),
                             reads=[PS(bk)], writes=[kkey])

            def gv(q, bv):
                for i in range(4):
                    t = 4 * q + i
                    for c in range(DC):
                        P.op("pe", (lambda e, i=i, t=t, c=c: e.matmul(
                            ps[bv][:, i * 128:(i + 1) * 128], lhsT=bufA[:, c, t * 128:(t + 1) * 128], rhs=wv[:, c, :],
                            start=(c == 0), stop=(c == DC - 1))),
                            reads=["wv", ("A", c, q)], writes=[PS(bv)])
                pv4 = ps[bv][:].rearrange("p (t h d) -> p t h d", t=4, h=2)
                d0 = V0[par][0][:, :, 0:64] if q == 0 else Vz[0][:, 4 * q:4 * q + 4, 0:64]
                d1 = V0[par][1][:, :, 64:128] if q == 0 else Vz[1][:, 4 * q:4 * q + 4, 64:128]
                vkey = ("V0", par) if q == 0 else ("V", q)
                P.op("act", (lambda e: e.copy(out=d0, in_=pv4[:, :, 0, :])), reads=[PS(bv)], writes=[vkey])
                P.op("dve", (lambda e: e.tensor_copy(out=d1, in_=pv4[:, :, 1, :])), reads=[PS(bv)], writes=[vkey])

            def items(tg, bank):
                tcs = slice(tg * 512, (tg + 1) * 512)
                out = []

                def mm_pair(w, key, c0):
                    def f():
                        for c in (c0, c0 + 1):
                            P.op("pe", (lambda e, c=c: e.matmul(ps[bank][:], lhsT=w[:, c, :], rhs=bufA[:, c, tcs],
                                                                start=(c == 0), stop=(c == DC - 1))),
                                 reads=[key, ("A", c, tg)], writes=[PS(bank)])
                    return f

                def q_evac():
                    for hf in range(2):
                        rs = slice(64 * hf, 64 * hf + 64)
                        dst = Q0[par][hf][rs, :] if tg == 0 else QTz[hf][rs, tcs]
                        if fox:
                            P.op("dve", (lambda e, rs=rs, dst=dst: e.tensor_scalar(out=dst, in0=ps[bank][rs, :],
                                                                                   scalar1=DH ** -0.5, scalar2=None,
                                                                                   op0=ALU.mult)),
                                 reads=[PS(bank)], writes=[Qk(par, tg)])
                        else:
                            P.op("act", (lambda e, rs=rs, dst=dst: e.mul(out=dst, in_=ps[bank][rs, :], mul=DH ** -0.5)),
                                 reads=[PS(bank)], writes=[Qk(par, tg)])

                def k_evac():
                    kkey = ("KT0", par) if tg == 0 else ("KT", tg)
                    if not fox:
                        dst = K0[par][0] if tg == 0 else KT[:, tcs]
                        P.op("dve", (lambda e: e.tensor_copy(out=dst, in_=ps[bank][:])), reads=[PS(bank)], writes=[kkey])
                    else:
                        for hf in range(2):
                            rs = slice(64 * hf, 64 * hf + 64)
                            dst = K0[par][hf][rs, :] if tg == 0 else KTz[hf][rs, tcs]
                            P.op("dve", (lambda e, rs=rs, dst=dst: e.tensor_copy(out=dst, in_=ps[bank][rs, :])),
                                 reads=[PS(bank)], writes=[kkey])

                def v_tile(i):
                    def f():
                        t = 4 * tg + i
                        for c in range(DC):
                            P.op("pe", (lambda e, c=c: e.matmul(
                                ps[bank][:, i * 128:(i + 1) * 128], lhsT=bufA[:, c, t * 128:(t + 1) * 128],
                                rhs=wv[:, c, :], start=(c == 0), stop=(c == DC - 1))),
                                reads=["wv", ("A", c, tg)], writes=[PS(bank)])
                    return f

                def v_evac():
                    pv4 = ps[bank][:].rearrange("p (t h d) -> p t h d", t=4, h=2)
                    d0 = V0[par][0][:, :, 0:64] if tg == 0 else Vz[0][:, 4 * tg:4 * tg + 4, 0:64]
                    d1 = V0[par][1][:, :, 64:128] if tg == 0 else Vz[1][:, 4 * tg:4 * tg + 4, 64:128]
                    vkey = ("V0", par) if tg == 0 else ("V", tg)
                    P.op("act", (lambda e: e.copy(out=d0, in_=pv4[:, :, 0, :])), reads=[PS(bank)], writes=[vkey])
                    P.op("dve", (lambda e: e.tensor_copy(out=d1, in_=pv4[:, :, 1, :])), reads=[PS(bank)], writes=[vkey])

                out += [mm_pair(wq, "wq", c0) for c0 in (0, 2, 4, 6)] + [q_evac]
                out += [mm_pair(wk, "wk", c0) for c0 in (0, 2, 4, 6)] + [k_evac]
                out += [v_tile(i) for i in range(4)] + [v_evac]
                return out

            return dict(prologue=prologue, aug=aug, gq=gq, gk=gk, gv=gv, items=items)

        def attn_all(npairs):
            steps = []
            for j in range(npairs):
                for qg in range(4):
                    for a in range(4 * qg + 3, -1, -1):
                        for half in range(2):
                            steps.append((j, half, qg, a))
            N = len(steps)
            later, cur = {}, [0]

            def info(n):
                j, half, qg, a = steps[n]
                lo = max(0, 128 * a - 512 * qg)
                amax = 4 * qg + 3
                return dict(j=j, par=j % 2, half=half, qg=qg, a=a, lo=lo, amax=amax, diag=(a >= 4 * qg),
                            base=64 * half, zs=n % (2 if fox else 3), es=n % 5, fs=n % 2, bs=n % 3, ws=n % 3, hg=2 * j + half)

            def kq(I):
                par, half, a, qg, lo = I["par"], I["half"], I["a"], I["qg"], I["lo"]
                return (Kap(par, half, a), Qap(par, half, qg, lo))

            def sb_z(n):
                I = info(n)
                zs, lo = I["zs"], I["lo"]
                kt, qt = kq(I)
                P.op("pe", (lambda e: e.matmul(ps[zs][:, lo:512], lhsT=kt, rhs=qt, start=True, stop=True)),
                     reads=[Kk(I["par"], I["a"]), Qk(I["par"], I["qg"]), "pad"], writes=[PS(zs)])

            def sb_act1(n):
                I = info(n)
                zs, lo, es, fs = I["zs"], I["lo"], I["es"], I["fs"]
                P.op("act", (lambda e: e.activation(out=ef[es][:, lo:512], in_=ps[zs][:, lo:512], func=AF.Exp)),
                     reads=[PS(zs)], writes=[("ef", es)])
                P.op("act", (lambda e: e.activation(out=spf[fs][:, lo:512], in_=ef[es][:, lo:512], func=AF.Ln,
                                                    bias=1.0)),
                     reads=[("ef", es)], writes=[("spf", fs)])

            def sb_dve1(n):
                I = info(n)
                zs, lo, es, fs, bs = I["zs"], I["lo"], I["es"], I["fs"], I["bs"]
                P.op("dve", (lambda e: e.tensor_tensor(out=ef[es][:, lo:512], in0=ps[zs][:, lo:512],
                                                       in1=spf[fs][:, lo:512], op=ALU.subtract)),
                     reads=[PS(zs), ("spf", fs)], writes=[("ef", es)])
                if I["diag"]:
                    P.op("dve", (lambda e: e.tensor_tensor(out=spb[bs][:, lo:lo + 128], in0=spf[fs][:, lo:lo + 128],
                                                           in1=mstrict, op=ALU.mult)),
                         reads=[("spf", fs), "mstrict"], writes=[("spb", bs)])
                    if lo + 128 < 512:
                        P.op("dve", (lambda e: e.tensor_copy(out=spb[bs][:, lo + 128:512],
                                                             in_=spf[fs][:, lo + 128:512])),
                             reads=[("spf", fs)], writes=[("spb", bs)])
                else:
                    P.op("dve", (lambda e: e.tensor_copy(out=spb[bs][:, lo:512], in_=spf[fs][:, lo:512])),
                         reads=[("spf", fs)], writes=[("spb", bs)])

            def sb_g1(n):
                I = info(n)
                lo, bs = I["lo"], I["bs"]
                gb = 3 + I["half"]
                P.op("pe", (lambda e: e.matmul(ps[gb][:, lo:512], lhsT=tri, rhs=spb[bs][:, lo:512],
                                               start=(I["a"] == I["amax"]), stop=False, skip_group_check=True)),
                     reads=["tri", ("spb", bs)], writes=[PS(gb)])

            def sb_d2(n):
                I = info(n)
                lo, es = I["lo"], I["es"]
                gb = 3 + I["half"]
                P.op("dve", (lambda e: e.tensor_tensor(out=ef[es][:, lo:512], in0=ef[es][:, lo:512],
                                                       in1=ps[gb][:, lo:512], op=ALU.subtract)),
                     reads=[("ef", es), PS(gb)], writes=[("ef", es)])

            def sb_g2(n):
                I = info(n)
                lo, bs, a = I["lo"], I["bs"], I["a"]
                gb = 3 + I["half"]
                if a == 0:
                    return
                P.op("pe", (lambda e: e.matmul(ps[gb][:, lo:512], lhsT=triu, rhs=spb[bs][:, lo:512], start=False,
                                               stop=(a == 1), skip_group_check=True)),
                     reads=["triu", ("spb", bs)], writes=[PS(gb)])

            def sb_w(n):
                I = info(n)
                lo, es, ws = I["lo"], I["es"], I["ws"]
                P.op("act", (lambda e: e.activation(out=wb[ws][:, lo:512], in_=ef[es][:, lo:512], func=AF.Exp)),
                     reads=[("ef", es)], writes=[("wb", ws)])

            def sb_mask(n):
                I = info(n)
                lo, ws = I["lo"], I["ws"]
                if I["diag"]:
                    P.op("dve", (lambda e: e.tensor_tensor(out=wb[ws][:, lo:lo + 128], in0=wb[ws][:, lo:lo + 128],
                                                           in1=mstrb, op=ALU.mult)),
                         reads=[("wb", ws), "mstrb"], writes=[("wb", ws)])

            def sb_pv(n):
                I = info(n)
                ws, lo, a, qg, half, j = I["ws"], I["lo"], I["a"], I["qg"], I["half"], I["j"]
                cs = slice(qg * 512, (qg + 1) * 512)
                ob = 5 + qg % 2
                P.op("pe", (lambda e: e.matmul(ps[ob][:, lo:512], lhsT=Vap(I["par"], half, a), rhs=wb[ws][:, lo:512],
                                               start=(a == I["amax"] and half == 0), stop=(a == 0 and half == 1),
                                               skip_group_check=True)),
                     reads=[Vk(I["par"], a), ("wb", ws), "pad"], writes=[PS(ob)])
                if a == 0 and half == 1:
                    later.setdefault(cur[0] + 1, []).append(lambda: P.op(
                        "dve", (lambda e: e.tensor_copy(out=mixT[:, j, cs], in_=ps[ob][:])),
                        reads=[PS(ob)], writes=[("mix", j, qg, 0), ("mix", j, qg, 1)]))

            def fx_z(n):
                I = info(n)
                zs, lo, qg, half = I["zs"], I["lo"], I["qg"], I["half"]
                kt, qt = kq(I)
                P.op("pe", (lambda e: e.matmul(ps[zs][:, lo:512], lhsT=kt, rhs=qt, start=True, stop=not I["diag"],
                                               skip_group_check=True)),
                     reads=[Kk(I["par"], I["a"]), Qk(I["par"], qg),
                            ("QTaug0", I["par"], half) if qg == 0 else ("QTaug", half), "pad"], writes=[PS(zs)])
                if I["diag"]:
                    P.op("pe", (lambda e: e.matmul(ps[zs][:, lo:lo + 128], lhsT=ident[:], rhs=negb, start=False,
                                                   stop=True, skip_group_check=True)),
                         reads=["ident", "negb"], writes=[PS(zs)])

            def fx_w(n):
                I = info(n)
                zs, lo, ws, a, hg = I["zs"], I["lo"], I["ws"], I["a"], I["hg"]
                bias = ncum[:, a, hg:hg + 1]
                P.op("act", (lambda e: e.activation(out=wb[ws][:, lo:512], in_=ps[zs][:, lo:512], func=AF.Exp,
                                                    bias=bias)),
                     reads=[PS(zs), ("ncum", a // 4)], writes=[("wb", ws)])

            def fx_pv(n):
                I = info(n)
                ws, lo, a, qg, half, j = I["ws"], I["lo"], I["a"], I["qg"], I["half"], I["j"]
                cs = slice(qg * 512, (qg + 1) * 512)
                ob = 3 + 2 * half + qg % 2
                P.op("pe", (lambda e: e.matmul(ps[ob][:, lo:512], lhsT=Vap(I["par"], half, a), rhs=wb[ws][:, lo:512],
                                               start=(a == I["amax"]), stop=(a == 0), skip_group_check=True)),
                     reads=[Vk(I["par"], a), ("wb", ws), "pad"], writes=[PS(ob)])
                if a == 0:
                    rr = 64 if half == 0 else 0
                    rs = slice(64 * half, 64 * half + 64)
                    rk, bk, hk = ("r_sb", half), ("bc_sb", half), ("r_hl", half)
                    M = 64 * (half + 1)

                    def t1():
                        P.op("act", (lambda e: e.activation(out=r_sb[half], in_=ps[ob][rr:rr + 1, :], func=AF.Ln)),
                             reads=[PS(ob)], writes=[rk])
                        P.op("act", (lambda e: e.activation(out=r_sb[half], in_=r_sb[half], func=AF.Exp, scale=-1.0)),
                             reads=[rk], writes=[rk])

                    def t2():
                        P.op("dve", (lambda e: e.tensor_copy(out=r_hi[half], in_=r_sb[half])),
                             reads=[rk], writes=[hk])
                        P.op("dve", (lambda e: e.tensor_tensor(out=r_lo[half], in0=r_sb[half], in1=r_hi[half],
                                                               op=ALU.subtract)),
                             reads=[rk, hk], writes=[hk])

                    def t3():
                        P.op("pe", (lambda e: e.matmul(ps[2][0:M, :], lhsT=onesb[rr:rr + 1, 0:M], rhs=r_hi[half],
                                                       start=True, stop=False, skip_group_check=True)),
                             reads=["onesb", hk], writes=[PS(2)])
                        P.op("pe", (lambda e: e.matmul(ps[2][0:M, :], lhsT=onesb[rr:rr + 1, 0:M], rhs=r_lo[half],
                                                       start=False, stop=True, skip_group_check=True)),
                             reads=["onesb", hk], writes=[PS(2)])

                    def t4():
                        P.op("dve", (lambda e: e.tensor_copy(out=bc_sb[half], in_=ps[2][rs, :])),
                             reads=[PS(2)], writes=[bk])

                    def t5():
                        P.op("dve", (lambda e: e.tensor_tensor(out=mixT[rs, j, cs], in0=ps[ob][rs, :], in1=bc_sb[half],
                                                               op=ALU.mult)),
                             reads=[PS(ob), bk], writes=[("mix", j, qg, half)])

                    for dl, fn in ((1, t1), (3, t2), (5, t3), (5, t4), (7, t5)):
                        later.setdefault(cur[0] + dl, []).append(fn)

            if not fox:
                sched = [(sb_z, 0), (sb_act1, 1), (sb_d2, 4), (sb_g1, 3), (sb_dve1, 2), (sb_w, 5), (sb_mask, 6),
                         (sb_pv, 7), (sb_g2, 4)]
            else:
                sched = [(fx_z, 0), (fx_w, 1), (fx_pv, 3)]
            depth = max(k for _, k in sched)
            bg = {}
            PG = [proj_groups(j) for j in range(npairs)]
            PG[0]["prologue"]()
            PG[0]["gq"](0, 0)
            PG[0]["gk"](0, 1)
            PG[0]["gv"](0, 2)
            def spread(fns, first, last):
                n = len(fns)
                for idx, f in enumerate(fns):
                    bg.setdefault(first + (idx * (last - first + 1)) // n, []).append(f)

            for j in range(npairs):
                B = 80 * j
                if fox:
                    bg.setdefault(B, []).append(lambda j=j: PG[j]["aug"](1))
                spread(PG[j]["items"](1, 7), B + 0, B + 7)
                spread(PG[j]["items"](2, 7), B + 8, B + 22)
                spread(PG[j]["items"](3, 7), B + 24, B + 38)
                if j + 1 < npairs:
                    bg.setdefault(B + 50, []).append(PG[j + 1]["prologue"])
                    spread(PG[j + 1]["items"](0, 7), B + 54, B + 68)
            i = 0
            while i < N + depth or later:
                cur[0] = i
                for fn in later.pop(i, []):
                    fn()
                for fn in bg.pop(i, []):
                    fn()
                for fn, k in sched:
                    if 0 <= i - k < N:
                        fn(i - k)
                i += 1

        if cfg.get("npairs", 8) < 8:
            P.op("pool", lambda e: e.memset(arena[:, 0:16384], 0.0),
                 writes=[("mix", j, qg, half) for j in range(8) for qg in range(4) for half in range(2)])
        attn_all(cfg.get("npairs", 8))

        if cfg.get("dbg"):
            MIXK = [("mix", j, qg, half) for j in range(8) for qg in range(4) for half in range(2)]
            dm = nc.dram_tensor("dbg_m", [128, 16384], BF16, kind="ExternalOutput").ap()
            P.dma("sp", lambda e: e.dma_start(out=dm, in_=arena[:, 0:16384]), reads=MIXK, writes=["dbg_m"], lane="dbgm")
            if fox:
                df = nc.dram_tensor("dbg_f", [128, 512], F32, kind="ExternalOutput").ap()
                db = nc.dram_tensor("dbg_b", [16, 4096], BF16, kind="ExternalOutput").ap()
                P.dma("sp", lambda e: e.dma_start(out=df, in_=farena[:, 3456:3456 + 512]),
                      reads=[("nlf", q) for q in range(4)] + [("ncum", q) for q in range(4)], writes=["dbg_f"], lane="dbgf")
                P.dma("sp", lambda e: e.dma_start(out=db, in_=xsf[0:16, 0:4096]),
                      reads=[("nhi", q) for q in range(4)] + [("nlo", q) for q in range(4)], writes=["dbg_b"], lane="dbgb")
                P.wait_all("sp", ["dbg_f", "dbg_b"])
            P.wait_all("sp", ["dbg_m"])
        wo = w_o_b if fox else w_o_a
        wov = wo.rearrange("(c p) n -> p c n", p=128)
        wos = [arena[:, 16384 + i * 4096:16384 + (i + 1) * 4096].rearrange("p (c n) -> p c n", c=DC) for i in range(2)]
        dead = ([("QT0", 0), ("KT0", 0), ("V0", 0), "pad"] + [("QT", q) for q in range(1, 4)]
                + [("KT", q) for q in range(1, 4)] + [("V", q) for q in range(1, 4)]
                + [("QTaug", hf) for hf in range(2)] + [("QTaug0", 0, hf) for hf in range(2)])
        for hf in range(2):
            P.dma("pool", (lambda e, hf=hf: e.dma_start(out=wos[hf], in_=wov[:, :, hf * 512:(hf + 1) * 512])),
                  reads=([("wos", 0)] if hf == 1 else []), writes=[("wos", hf)] + dead, lane="wos%d" % hf)
        xs_alias = [("QT0", 1), ("KT0", 1), ("V0", 1)] + ([("nhi", q) for q in range(4)] + [("nlo", q) for q in range(4)]
                                                         if fox else [])
        n = 0
        fold = cfg.get("nexp", NE) > 0
        for tg in range(4):
            for hf in range(2):
                for i in range(4):
                    t = 4 * tg + i
                    k = n % 4
                    n += 1
                    for c in range(DC):
                        P.op("pe", (lambda e, k=k, c=c, t=t, hf=hf: e.matmul(
                            ps[k][:], lhsT=mixT[:, c, t * 128:(t + 1) * 128], rhs=wos[hf][:, c, :],
                            start=(c == 0), stop=(c == DC - 1))),
                            reads=[("mix", c, t // 4, 0), ("mix", c, t // 4, 1), ("wos", hf)], writes=[PS(k)])
                    hv = h[:, t, hf * 512:(hf + 1) * 512]
                    P.op("dve", (lambda e, k=k, hv=hv: e.tensor_tensor(out=hv, in0=hv, in1=ps[k][:], op=ALU.add)),
                         reads=[PS(k), ("h", t)], writes=[("h", t)])
            if fold:
                if tg >= 1:
                    norm_group_b(tg - 1, 3 + layer, banks=(4, 5, 6, 7))
                norm_group_a(tg, extra_w=xs_alias)
        if fold:
            norm_group_b(3, 3 + layer, banks=(4, 5, 6, 7))

    def moe(layer, nexp, tail_cb=None, head_cb=None):
        P.barrier()
        BGQ = []
        wg = [arena[:, i * 4096:(i + 1) * 4096].rearrange("p (c f) -> p c f", c=DC) for i in range(2)]
        wu = [arena[:, 8192 + i * 4096:8192 + (i + 1) * 4096].rearrange("p (c f) -> p c f", c=DC) for i in range(2)]
        wd = [arena[:, 16384 + i * 4096:16384 + (i + 1) * 4096].rearrange("p (c f) -> p c f", c=4) for i in range(2)]
        hT = [arena[:, 24576 + i * 2048:24576 + (i + 1) * 2048].rearrange("p (c f) -> p c f", c=4) for i in range(2)]
        wr = arena[:, 28672:28672 + 160].rearrange("p (c f) -> p c f", c=DC)
        sg = [farena[:, i * 512:(i + 1) * 512] for i in range(2)]
        rl = farena[:, 1024:1024 + NT * 20].rearrange("p (t f) -> p t f", t=NT)
        comb = farena[:, 1344:1344 + NT * NE].rearrange("p (t f) -> p t f", t=NT)
        o = 1600
        rt = {}
        for nm, w in (("gmax", 1), ("gsh", 4), ("gex", 4), ("gsum", 1), ("gprob", 1), ("gm", 4), ("pen", 4),
                      ("msk", 16), ("m1", 1), ("oh1", 16), ("msk2", 16), ("m2", 1), ("oh2", 16), ("dm", 1),
                      ("ed", 1), ("den", 1), ("w1", 1), ("w2", 1), ("c1", 16)):
            rt[nm] = farena[:, o:o + NT * w].rearrange("p (t f) -> p t f", t=NT)
            o += NT * w
        assert o <= 4608

        if not cfg.get("attn", True):
            norm_transpose(3 + layer)
        wgv = w_group[layer].rearrange("(c p) n -> p c n", p=128)
        wrv = w_router[layer].rearrange("(c p) n -> p c n", p=128)
        wr32 = farena[:, 4608:4768].rearrange("p (c f) -> p c f", c=DC)
        P.dma("sp", (lambda e: e.dma_start(out=wr32[:, :, 0:4], in_=wgv)), writes=["wr32a"], lane="wra")
        P.dma("sp", (lambda e: e.dma_start(out=wr32[:, :, 4:20], in_=wrv)), writes=["wr32b"], lane="wrb")
        P.op("dve", lambda e: e.tensor_copy(out=wr, in_=wr32), reads=["wr32a", "wr32b"], writes=["wr_a", "wr_b"])
        for t in range(NT):
            k = 4 + t % 2
            for c in range(DC):
                P.op("pe", (lambda e, k=k, t=t, c=c: e.matmul(
                    ps[k][:, 0:20], lhsT=bufA[:, c, t * 128:(t + 1) * 128], rhs=wr[:, c, :],
                    start=(c == 0), stop=(c == DC - 1))),
                    reads=[("A", c, t // 4), "wr_a", "wr_b"], writes=[PS(k)])
            P.op("act", (lambda e, k=k, t=t: e.copy(out=rl[:, t, :], in_=ps[k][:, 0:20])),
                 reads=[PS(k)], writes=[("rl", t)])
        RL = [("rl", t) for t in range(NT)]

        def dv(fn, reads, writes):
            P.op("dve", fn, reads=reads, writes=writes)

        def bc(ap, w):
            return ap.to_broadcast([128, NT, w])

        gl = rl[:, :, 0:4]
        el = rl[:, :, 4:20]
        AXX = mybir.AxisListType.X
        dv(lambda e: e.tensor_reduce(out=rt["gmax"], in_=gl, axis=AXX, op=ALU.max), RL, ["gmax"])
        dv(lambda e: e.tensor_tensor(out=rt["gsh"], in0=gl, in1=bc(rt["gmax"], 4), op=ALU.subtract),
           RL + ["gmax"], ["gsh"])
        P.op("act", lambda e: e.activation(out=rt["gex"], in_=rt["gsh"], func=AF.Exp), reads=["gsh"], writes=["gex"])
        dv(lambda e: e.tensor_reduce(out=rt["gsum"], in_=rt["gex"], axis=AXX, op=ALU.add), ["gex"], ["gsum"])
        dv(lambda e: e.reciprocal(out=rt["gprob"], in_=rt["gsum"]), ["gsum"], ["gprob"])
        dv(lambda e: e.tensor_tensor(out=rt["gm"], in0=gl, in1=bc(rt["gmax"], 4), op=ALU.is_equal),
           RL + ["gmax"], ["gm"])
        dv(lambda e: e.tensor_scalar(out=rt["pen"], in0=rt["gm"], scalar1=-1.0, scalar2=1e9,
                                     op0=ALU.add, op1=ALU.mult), ["gm"], ["pen"])
        dv(lambda e: e.tensor_tensor(
            out=rt["msk"].rearrange("p t (g x) -> p t g x", g=4), in0=el.rearrange("p t (g x) -> p t g x", g=4),
            in1=rt["pen"].unsqueeze(3).to_broadcast([128, NT, 4, 4]), op=ALU.add), RL + ["pen"], ["msk"])
        dv(lambda e: e.tensor_reduce(out=rt["m1"], in_=rt["msk"], axis=AXX, op=ALU.max), ["msk"], ["m1"])
        dv(lambda e: e.tensor_tensor(out=rt["oh1"], in0=rt["msk"], in1=bc(rt["m1"], 16), op=ALU.is_equal),
           ["msk", "m1"], ["oh1"])
        dv(lambda e: e.scalar_tensor_tensor(out=rt["msk2"], in0=rt["oh1"], scalar=-1e9, in1=rt["msk"],
                                            op0=ALU.mult, op1=ALU.add), ["oh1", "msk"], ["msk2"])
        dv(lambda e: e.tensor_reduce(out=rt["m2"], in_=rt["msk2"], axis=AXX, op=ALU.max), ["msk2"], ["m2"])
        dv(lambda e: e.tensor_tensor(out=rt["oh2"], in0=rt["msk2"], in1=bc(rt["m2"], 16), op=ALU.is_equal),
           ["msk2", "m2"], ["oh2"])
        dv(lambda e: e.tensor_tensor(out=rt["dm"], in0=rt["m2"], in1=rt["m1"], op=ALU.subtract),
           ["m1", "m2"], ["dm"])
        P.op("act", lambda e: e.activation(out=rt["ed"], in_=rt["dm"], func=AF.Exp), reads=["dm"], writes=["ed"])
        dv(lambda e: e.tensor_scalar(out=rt["den"], in0=rt["ed"], scalar1=1.0, scalar2=None, op0=ALU.add),
           ["ed"], ["den"])
        dv(lambda e: e.reciprocal(out=rt["w1"], in_=rt["den"]), ["den"], ["w1"])
        dv(lambda e: e.tensor_tensor(out=rt["w1"], in0=rt["w1"], in1=rt["gprob"], op=ALU.mult),
           ["w1", "gprob"], ["w1"])
        dv(lambda e: e.tensor_tensor(out=rt["w2"], in0=rt["w1"], in1=rt["ed"], op=ALU.mult),
           ["w1", "ed"], ["w2"])
        dv(lambda e: e.tensor_tensor(out=rt["c1"], in0=rt["oh1"], in1=bc(rt["w1"], 16), op=ALU.mult),
           ["oh1", "w1"], ["c1"])
        dv(lambda e: e.tensor_tensor(out=comb, in0=rt["oh2"], in1=bc(rt["w2"], 16), op=ALU.mult),
           ["oh2", "w2"], ["comb"])
        dv(lambda e: e.tensor_tensor(out=comb, in0=comb, in1=rt["c1"], op=ALU.add), ["comb", "c1"], ["comb"])

        def load_w(ex):
            s = ex % 2
            gv = w_gate[layer, ex].rearrange("(c p) f -> p c f", p=128)
            uv = w_up[layer, ex].rearrange("(c p) f -> p c f", p=128)
            dvw = w_down[layer, ex].rearrange("(c p) f -> p c f", p=128)
            P.dma("pool", (lambda e: e.dma_start(out=wg[s], in_=gv)), writes=[("wg", s)], lane="wg%d" % s)
            P.dma("pool", (lambda e: e.dma_start(out=wu[s], in_=uv)), writes=[("wu", s)], lane="wu%d" % s)
            P.dma("pool", (lambda e: e.dma_start(out=wd[s], in_=dvw)), writes=[("wd", s)], lane="wd%d" % s)

        def gate_up(ex, tg, hs):
            s = ex % 2
            for fc in range(4):
                kg, ku = fc % 2, 2 + fc % 2
                for c in range(DC):
                    P.op("pe", (lambda e, kg=kg, c=c, fc=fc: e.matmul(
                        ps[kg][:], lhsT=wg[s][:, c, fc * 128:(fc + 1) * 128], rhs=bufA[:, c, tg * 512:(tg + 1) * 512],
                        start=(c == 0), stop=(c == DC - 1))),
                        reads=[("wg", s), ("A", c, tg)], writes=[PS(kg)])
                for c in range(DC):
                    P.op("pe", (lambda e, ku=ku, c=c, fc=fc: e.matmul(
                        ps[ku][:], lhsT=wu[s][:, c, fc * 128:(fc + 1) * 128], rhs=bufA[:, c, tg * 512:(tg + 1) * 512],
                        start=(c == 0), stop=(c == DC - 1))),
                        reads=[("wu", s), ("A", c, tg)], writes=[PS(ku)])
                P.op("act", (lambda e, kg=kg, fc=fc: e.activation(out=sg[fc % 2], in_=ps[kg][:], func=AF.Silu)),
                     reads=[PS(kg)], writes=[("sg", fc % 2)])
                P.op("dve", (lambda e, ku=ku, fc=fc: e.tensor_tensor(
                    out=hT[hs][:, fc, :], in0=sg[fc % 2], in1=ps[ku][:], op=ALU.mult)),
                    reads=[("sg", fc % 2), PS(ku)], writes=[("hT", hs, fc)])
                if BGQ:
                    BGQ.pop(0)()

        def down(ex, tg, hs):
            s = ex % 2
            n = 0
            for i in range(4):
                t = 4 * tg + i
                for half in range(2):
                    k = 4 + n % 3
                    n += 1
                    for fc in range(4):
                        P.op("pe", (lambda e, k=k, fc=fc, i=i, half=half: e.matmul(
                            ps[k][:], lhsT=hT[hs][:, fc, i * 128:(i + 1) * 128],
                            rhs=wd[s][:, fc, half * 512:(half + 1) * 512], start=(fc == 0), stop=(fc == 3))),
                            reads=[("hT", hs, fc), ("wd", s)], writes=[PS(k)])
                    hv = h[:, t, half * 512:(half + 1) * 512]
                    P.op("dve", (lambda e, k=k, hv=hv, t=t: e.scalar_tensor_tensor(
                        out=hv, in0=ps[k][:], scalar=comb[:, t, ex:ex + 1], in1=hv, op0=ALU.mult, op1=ALU.add)),
                        reads=[PS(k), "comb", ("h", t)], writes=[("h", t)])
                    if BGQ:
                        BGQ.pop(0)()

        steps = [(ex, tg) for ex in range(nexp) for tg in range(4)]
        load_w(0)
        if head_cb is not None:
            head_cb()
        prev = None
        for n, (ex, tg) in enumerate(steps):
            gate_up(ex, tg, n % 2)
            if prev is not None:
                down(prev[0], prev[1], (n - 1) % 2)
                if tail_cb is not None and prev[0] == nexp - 1:
                    tail_cb(prev[1], BGQ)
            if tg == 0 and ex + 1 < nexp:
                load_w(ex + 1)
            prev = (ex, tg)
        down(prev[0], prev[1], (len(steps) - 1) % 2)
        if tail_cb is not None:
            tail_cb(prev[1], BGQ)
        while BGQ:
            BGQ.pop(0)()

    xs32 = xs[:].rearrange("p s a d -> p (s a d)").bitcast(F32)
    gfin = xs32[:, 0:D]
    ost = [xs32[:, (1 + i) * D:(2 + i) * D] for i in range(2)]
    ov = out.rearrange("(t p) d -> p t d", p=128)

    def final_setup():
        P.dma("sp", lambda e: e.dma_start(out=gfin, in_=final_norm.partition_broadcast(128)),
              writes=["gfin"] + [("xs", sl, i) for sl in range(2) for i in range(4)], lane="gfin")

    def final_group(tg, bgq=None):
        norm_stats(4 * tg, 4 * tg + 4)
        for i in range(4):
            t = 4 * tg + i
            s = t % 2
            P.op("dve", (lambda e, t=t, s=s: e.scalar_tensor_tensor(
                out=ost[s], in0=h[:, t, :], scalar=rstd[:, t:t + 1], in1=gfin, op0=ALU.mult, op1=ALU.mult)),
                reads=[("h", t), ("rstd", t), "gfin"], writes=[("ost", s)])
            P.dma("sp", (lambda e, t=t, s=s: e.dma_start(out=ov[:, t, :], in_=ost[s])),
                  reads=[("ost", s)], writes=[("out", t)], lane="o%d" % s)

    def fox_norm_cb(tg, bgq):
        while bgq:
            bgq.pop(0)()
        norm_group_a(tg)
        for c in range(DC):
            bgq.append(lambda c=c: norm_group_b(tg, None, banks=(7,), chunks=[c]))

    layers = cfg["layers"]
    nexp = cfg.get("nexp", NE)
    for li, layer in enumerate(layers):
        last = (li == len(layers) - 1)
        if cfg.get("attn", True):
            attention(layer, prenormed=(li > 0 and nexp > 0 and layer == 1))
        if nexp > 0:
            if last:
                moe(layer, nexp, tail_cb=final_group, head_cb=final_setup)
            elif cfg.get("attn", True) and layers[li + 1] == 1:
                moe(layer, nexp, tail_cb=fox_norm_cb)
            else:
                moe(layer, nexp)
    if nexp == 0:
        P.barrier()
        final_setup()
        for tg in range(4):
            final_group(tg)
    P.wait_all("sp", [("out", t) for t in range(NT)])
    P.emit(nc)
    es.close()
    return nc


_CACHE = {}


def _prep(inputs):
    f = lambda a: np.ascontiguousarray(np.asarray(a, dtype=np.float32))
    shared = {
        "attn_norm": f(inputs["attn_norm"]),
        "w_qkv_a": f(inputs["w_qkv_a"])[0],
        "w_o_a": f(inputs["w_o_a"])[0],
        "kv_norm": f(inputs["kv_norm"]).reshape(1, D),
        "w_kvf": f(inputs["w_kvf"]),
        "b_f": f(inputs["b_f"]).reshape(1, H),
        "w_q_b": f(inputs["w_q_b"])[0],
        "w_o_b": f(inputs["w_o_b"])[0],
        "moe_norm": f(inputs["moe_norm"]),
        "w_group": f(inputs["w_group"]),
        "w_router": f(inputs["w_router"]),
        "w_gate": f(inputs["w_gate"]),
        "w_up": f(inputs["w_up"]),
        "w_down": f(inputs["w_down"]),
        "final_norm": f(inputs["final_norm"]).reshape(1, D),
    }
    xin = f(inputs["x"])
    return [dict(shared, x=xin[b]) for b in range(xin.shape[0])]


def run_cfg(inputs, cfg, trace=False):
    nc = bass.Bass("TRN2", target_bir_lowering=False)
    build(nc, cfg)
    in_maps = _prep(inputs)
    res = run_bass_kernel_spmd(nc, in_maps, core_ids=list(range(len(in_maps))), trace=trace)
    outs = np.stack([np.asarray(r["out"]) for r in res.results], axis=0)
    if cfg.get("dbg"):
        res.dbg = {k: np.asarray(v) for k, v in res.results[0].items() if k.startswith("dbg")}
    return outs, res


def kernel(**inputs):
    outs, _ = run_cfg(inputs, dict(layers=[0, 1], attn=True, nexp=NE))
    return outs.astype(np.float32)
```
